# Optimizing a Trainium2 kernel written in Bass

```python
import math
import jax, jax.numpy as jnp
from jax import lax
import numpy as np

D_MODEL = 1024
BATCH = 8
SEQ = 2048
DEPTH = 1

RET_HEADS = 4
RET_DK = 128
RET_DV = 128
RET_CHUNK = 128
RET_FWD_DECAY_OFFSET = 5.0
RET_BWD_DECAY_OFFSET = 5.5
RET_THETA_BASE = 10000.0
DIFF_HEADS = 4
DIFF_DH = 64
DIFF_DV = 2 * DIFF_DH
Q_BLOCK = 128
N_BUCKETS = 32
MAX_DISTANCE = 128
N_EXPERTS = 16
EXPERT_FF = 1024
CAPACITY_FACTOR = 2
NORM_EPS = 1e-6

RET_QK_W = RET_HEADS * RET_DK
RET_V_W = RET_HEADS * RET_DV
DIFF_QK_W = DIFF_HEADS * DIFF_DH
DIFF_V_W = DIFF_HEADS * DIFF_DV
N_BRANCHES = 2
IN_SIZES = (RET_QK_W, RET_QK_W, RET_V_W, RET_V_W,
            DIFF_QK_W, DIFF_QK_W, DIFF_QK_W, DIFF_QK_W, DIFF_V_W,
            D_MODEL, D_MODEL)
IN_COLS = sum(IN_SIZES)

kernel_name = 'hybrid_retention_diffattn_ec_moe'


def _rmsnorm(x, g):
    xf = x.astype(jnp.float32)
    y = xf * lax.rsqrt(jnp.mean(xf * xf, axis=-1, keepdims=True) + NORM_EPS)
    return (y * g.astype(jnp.float32)).astype(x.dtype)


def _rotate(x, pos):
    d = x.shape[-1]
    inv = 1.0 / (RET_THETA_BASE ** jnp.linspace(0.0, 1.0, d // 2, dtype=jnp.float32))
    ang = pos.astype(jnp.float32)[:, None] * inv[None, :]
    cos = jnp.cos(ang)[None, :, None, :].astype(x.dtype)
    sin = jnp.sin(ang)[None, :, None, :].astype(x.dtype)
    x1 = x[..., 0::2]
    x2 = x[..., 1::2]
    return jnp.stack([x1 * cos - x2 * sin, x1 * sin + x2 * cos], axis=-1).reshape(x.shape)


def _retention_one_dir(q, k, v, log_g, include_diag):
    B, H, S, dk = q.shape
    dv = v.shape[-1]
    C = RET_CHUNK
    N = S // C
    qc = q.reshape(B, H, N, C, dk)
    kc = k.reshape(B, H, N, C, dk)
    vc = v.reshape(B, H, N, C, dv)
    idx = jnp.arange(C, dtype=jnp.float32)
    diff = idx[:, None] - idx[None, :]
    mask = (diff >= 0) if include_diag else (diff > 0)
    decay_in = jnp.where(mask[None], jnp.exp(jnp.maximum(diff, 0.0)[None] * log_g[:, None, None]), 0.0)
    decay_in = decay_in.astype(q.dtype)
    scores = jnp.einsum('bhncd,bhnmd->bhncm', qc, kc) * decay_in[None, :, None]
    inner = jnp.einsum('bhncm,bhnme->bhnce', scores, vc)
    k_dec = jnp.exp((C - 1 - idx)[None, :] * log_g[:, None]).astype(q.dtype)
    q_dec = jnp.exp((idx + 1)[None, :] * log_g[:, None]).astype(q.dtype)
    chunk_dec = jnp.exp(C * log_g)
    kv = jnp.einsum('bhncd,hc,bhnce->nbhde', kc, k_dec, vc).astype(jnp.float32)

    def step(state, kv_n):
        return chunk_dec[None, :, None, None] * state + kv_n, state

    _, prev = lax.scan(step, jnp.zeros((B, H, dk, dv), jnp.float32), kv)
    cross = jnp.einsum('bhncd,hc,nbhde->bhnce', qc, q_dec, prev.astype(q.dtype))
    return (inner + cross).reshape(B, H, S, dv)


def _t5_bucket(rel):
    nb = N_BUCKETS // 2
    max_exact = nb // 2
    ret = (rel > 0).astype(jnp.int32) * nb
    n = jnp.abs(rel)
    large = max_exact + (jnp.log(jnp.maximum(n, 1).astype(jnp.float32) / max_exact)
                         / math.log(MAX_DISTANCE / max_exact) * (nb - max_exact)).astype(jnp.int32)
    large = jnp.minimum(large, nb - 1)
    return ret + jnp.where(n < max_exact, n, large)


def _diff_attention(q1, q2, k1, k2, v, rel_bias, lam):
    B, H, S, dh = q1.shape
    dv = v.shape[-1]
    NB = S // Q_BLOCK
    scale = dh ** -0.5
    kpos = jnp.arange(S, dtype=jnp.int32)

    def to_blocks(t):
        return jnp.moveaxis(t.reshape(B, H, NB, Q_BLOCK, dh), 2, 0)

    def block(args):
        q1b, q2b, start = args
        qpos = start + jnp.arange(Q_BLOCK, dtype=jnp.int32)
        bias = jnp.transpose(rel_bias[_t5_bucket(kpos[None, :] - qpos[:, None])], (2, 0, 1))
        bias = bias.astype(jnp.float32)
        s1 = jnp.einsum('bhqd,bhkd->bhqk', q1b, k1).astype(jnp.float32) * scale + bias
        s2 = jnp.einsum('bhqd,bhkd->bhqk', q2b, k2).astype(jnp.float32) * scale + bias
        attn = jax.nn.softmax(s1, axis=-1) - lam * jax.nn.softmax(s2, axis=-1)
        return jnp.einsum('bhqk,bhke->bhqe', attn.astype(v.dtype), v)

    starts = jnp.arange(NB, dtype=jnp.int32) * Q_BLOCK
    out = lax.map(block, (to_blocks(q1), to_blocks(q2), starts))
    return jnp.transpose(out, (1, 0, 3, 2, 4)).reshape(B, S, H, dv)


def _expert_choice(h, w_router, w_gate, w_up, w_down):
    B, S, D = h.shape
    cap = CAPACITY_FACTOR * S // N_EXPERTS
    aff = jax.nn.softmax((h @ w_router).astype(jnp.float32), axis=-1)
    g, idx = lax.top_k(jnp.swapaxes(aff, 1, 2), cap)
    bidx = jnp.arange(B)[:, None, None]
    xin = h[bidx, idx]
    a = jnp.einsum('becd,edf->becf', xin, w_gate)
    u = jnp.einsum('becd,edf->becf', xin, w_up)
    y = jnp.einsum('becf,efd->becd', jax.nn.silu(a) * u, w_down) * g[..., None].astype(h.dtype)
    return jnp.zeros_like(h).at[bidx, idx].add(y)


def setup_inputs(seed: int = 0) -> dict:
    key = jax.random.key(seed)
    ks = jax.random.split(key, 24)
    f32 = jnp.float32
    nrm = lambda k, shape, s: jax.random.normal(k, shape, f32) * s
    gain = lambda k, shape: 1.0 + 0.02 * jax.random.normal(k, shape, f32)
    L, D = DEPTH, D_MODEL
    return {
        'x': nrm(ks[0], (BATCH, SEQ, D), 1.0),
        'c': nrm(ks[1], (BATCH, D), 1.0),
        'w_ada': nrm(ks[2], (L, D, 6 * D), D ** -0.5),
        'b_ada': nrm(ks[3], (L, 6 * D), 0.01),
        'norm_mix_g': gain(ks[4], (L, D)),
        'w_in': nrm(ks[5], (L, D, IN_COLS), D ** -0.5),
        'ret_gn_g': gain(ks[6], (L, RET_V_W)),
        'diff_subln_g': gain(ks[7], (L, DIFF_DV)),
        'lambda_q1': nrm(ks[8], (L, DIFF_DH), 0.1),
        'lambda_k1': nrm(ks[9], (L, DIFF_DH), 0.1),
        'lambda_q2': nrm(ks[10], (L, DIFF_DH), 0.1),
        'lambda_k2': nrm(ks[11], (L, DIFF_DH), 0.1),
        'w_ret_out': nrm(ks[12], (L, RET_V_W, D), RET_V_W ** -0.5),
        'w_diff_out': nrm(ks[13], (L, DIFF_V_W, D), DIFF_V_W ** -0.5),
        'w_o': nrm(ks[14], (L, D, D), D ** -0.5),
        'rel_bias': nrm(ks[15], (N_BUCKETS, DIFF_HEADS), 0.5),
        'norm_ffn_g': gain(ks[16], (L, D)),
        'w_router': nrm(ks[17], (L, D, N_EXPERTS), D ** -0.5),
        'w_exp_gate': nrm(ks[18], (L, N_EXPERTS, D, EXPERT_FF), D ** -0.5),
        'w_exp_up': nrm(ks[19], (L, N_EXPERTS, D, EXPERT_FF), D ** -0.5),
        'w_exp_down': nrm(ks[20], (L, N_EXPERTS, EXPERT_FF, D), EXPERT_FF ** -0.5),
        'final_g': gain(ks[21], (D,)),
    }


def reference(x, c, w_ada, b_ada, norm_mix_g, w_in, ret_gn_g, diff_subln_g,
              lambda_q1, lambda_k1, lambda_q2, lambda_k2, w_ret_out, w_diff_out,
              w_o, rel_bias, norm_ffn_g, w_router, w_exp_gate, w_exp_up,
              w_exp_down, final_g):
    B, S, D = x.shape
    f32 = jnp.float32
    pos = jnp.arange(S, dtype=jnp.int32)
    heads = jnp.arange(RET_HEADS, dtype=f32)
    log_g_fwd = jnp.log1p(-jnp.exp2(-RET_FWD_DECAY_OFFSET - heads))
    log_g_bwd = jnp.log1p(-jnp.exp2(-RET_BWD_DECAY_OFFSET - heads))
    split_points = np.cumsum(IN_SIZES)[:-1].tolist()
    to_bhsd = lambda t: jnp.transpose(t, (0, 2, 1, 3))

    for l in range(DEPTH):
        mod = jax.nn.silu(c) @ w_ada[l] + b_ada[l]
        sh1, sc1, ga1, sh2, sc2, ga2 = [m[:, None, :] for m in jnp.split(mod, 6, axis=-1)]

        h = _rmsnorm(x, norm_mix_g[l]) * (1.0 + sc1) + sh1
        proj = h @ w_in[l]
        rq, rk, rv, rg, dq1, dq2, dk1, dk2, dvv, gr, gd = jnp.split(proj, split_points, axis=-1)

        rq = _rotate(rq.reshape(B, S, RET_HEADS, RET_DK), pos)
        rk = _rotate(rk.reshape(B, S, RET_HEADS, RET_DK), pos) * (RET_DK ** -0.5)
        rq, rk = to_bhsd(rq), to_bhsd(rk)
        rv = to_bhsd(rv.reshape(B, S, RET_HEADS, RET_DV))
        fwd = _retention_one_dir(rq, rk, rv, log_g_fwd, True)
        bwd = jnp.flip(_retention_one_dir(jnp.flip(rq, 2), jnp.flip(rk, 2), jnp.flip(rv, 2),
                                          log_g_bwd, False), 2)
        yr = to_bhsd(fwd + bwd).astype(f32)
        mu = jnp.mean(yr, axis=-1, keepdims=True)
        var = jnp.mean(jnp.square(yr - mu), axis=-1, keepdims=True)
        yr = ((yr - mu) * lax.rsqrt(var + NORM_EPS)).reshape(B, S, RET_V_W)
        yr = (yr * ret_gn_g[l].astype(f32)).astype(x.dtype)
        y_ret = jax.nn.silu(rg) * yr

        lam_init = 0.8 - 0.6 * math.exp(-0.3 * l)
        lam = (jnp.exp(jnp.sum(lambda_q1[l].astype(f32) * lambda_k1[l].astype(f32)))
               - jnp.exp(jnp.sum(lambda_q2[l].astype(f32) * lambda_k2[l].astype(f32))) + lam_init)
        hd = lambda t, d: to_bhsd(t.reshape(B, S, DIFF_HEADS, d))
        yd = _diff_attention(hd(dq1, DIFF_DH), hd(dq2, DIFF_DH), hd(dk1, DIFF_DH),
                             hd(dk2, DIFF_DH), hd(dvv, DIFF_DV), rel_bias, lam)
        y_diff = (_rmsnorm(yd, diff_subln_g[l]) * (1.0 - lam_init)).reshape(B, S, DIFF_V_W)

        merged = (jax.nn.sigmoid(gr) * (y_ret @ w_ret_out[l])
                  + jax.nn.sigmoid(gd) * (y_diff @ w_diff_out[l]))
        x = x + ga1 * (merged @ w_o[l])

        h = _rmsnorm(x, norm_ffn_g[l]) * (1.0 + sc2) + sh2
        x = x + ga2 * _expert_choice(h, w_router[l], w_exp_gate[l], w_exp_up[l], w_exp_down[l])

    return _rmsnorm(x, final_g)
```

```python
import math
import numpy as np
import ml_dtypes
from contextlib import ExitStack
import concourse.bass as bass
import concourse.mybir as mybir
from concourse.bass_utils import run_bass_kernel_spmd

F32 = mybir.dt.float32
BF16 = mybir.dt.bfloat16
I32 = mybir.dt.int32
AF = mybir.ActivationFunctionType
ALU = mybir.AluOpType
AX = mybir.AxisListType

D = 1024
S_LEN = 2048
NT = 16
NE = 16
CAP = 256
EPS = 1e-6


class _St:
    __slots__ = ("w", "r")

    def __init__(self):
        self.w = None
        self.r = {}


class Tile:
    def __init__(self, h, name, psum=False):
        self.h = h
        self.name = name
        self.psum = psum
        self.whole = _St()
        self.parts = {}

    def __getitem__(self, idx):
        return self.h[idx]

    def states(self, key):
        if key is None:
            return [self.whole] + list(self.parts.values())
        if key not in self.parts:
            self.parts[key] = _St()
        return [self.whole, self.parts[key]]

    def target(self, key):
        if key is None:
            return self.whole
        if key not in self.parts:
            self.parts[key] = _St()
        return self.parts[key]


class Op:
    __slots__ = ("id", "eng", "dma", "sig", "semval", "sem")


import os
_SKIP = set(os.environ.get('KSKIP', '').split(','))


class Sched:
    ENGS = ("pe", "act", "dve", "pool", "sp")

    def __init__(self, nc, ndma_sems=12):
        self.nc = nc
        self.ops = []
        self.stack = ExitStack()
        self.ndma = ndma_sems
        self.freed = []
        self.scopes = []
        st = self.stack
        self.esem = {e: st.enter_context(nc.semaphore("s_" + e)) for e in self.ENGS}
        self.dsem = {e: [st.enter_context(nc.semaphore("d_%s_%d" % (e, i))) for i in range(ndma_sems)]
                     for e in ("sp", "pool", "act")}
        self.cnt = {e: 0 for e in self.ENGS}
        self.dcnt = {e: 0 for e in ("sp", "pool", "act")}
        self.known = {e: {} for e in self.ENGS}
        self.engobj = {"pe": nc.tensor, "act": nc.scalar, "dve": nc.vector, "pool": nc.gpsimd, "sp": nc.sync}
        self.n_waits = 0
        self.pending_pe = []

    def sb(self, name, shape, dtype):
        h = self._ctx().enter_context(self.nc.sbuf_tensor(name, list(shape), dtype))
        return self._mk(h, name)

    def ps(self, name, shape, dtype):
        assert list(shape) == [128, 512] and dtype == F32
        h = self._ctx().enter_context(self.nc.psum_tensor(name, list(shape), dtype))
        return self._mk(h, name, True)

    def _mk(self, h, name, psum=False):
        t = Tile(h, name, psum)
        for (e, oid) in self.freed:
            if e not in t.whole.r or t.whole.r[e] < oid:
                t.whole.r[e] = oid
        if self.scopes:
            self.scopes[-1][1].append(t)
        return t

    def _ctx(self):
        return self.scopes[-1][0] if self.scopes else self.stack

    def push(self):
        self.scopes.append((ExitStack(), []))

    def pop(self):
        es, tiles = self.scopes.pop()
        latest = {}
        keep = []
        for (e, oid) in self.freed:
            if e.startswith("dma"):
                keep.append((e, oid))
            else:
                latest[e] = max(latest.get(e, -1), oid)
        for t in tiles:
            for st in [t.whole] + list(t.parts.values()):
                ids = ([st.w] if st.w is not None else []) + list(st.r.values())
                for oid in ids:
                    o = self.ops[oid]
                    if o.dma:
                        keep.append(("dma%d" % oid, oid))
                    else:
                        latest[o.eng] = max(latest.get(o.eng, -1), oid)
        keep = list({k: v for k, v in keep}.items())
        if len(keep) > 48:
            keep = keep[-48:]
        self.freed = list(latest.items()) + keep
        es.close()

    def op(self, eng, fn, reads=(), writes=(), dma=False, sig=True):
        o = Op()
        o.id = len(self.ops)
        o.eng = eng
        o.dma = dma
        o.sig = sig or dma
        o.sem = None
        o.semval = None
        deps = set()
        key_e = ("dma%d" % o.id) if dma else eng

        def norm(x):
            return x if isinstance(x, tuple) else (x, None)

        if eng != "pe":
            extra = [norm(x)[0] for x in reads if norm(x)[0].psum]
            writes = list(writes) + extra
        for x in reads:
            t, k = norm(x)
            for st in t.states(k):
                if st.w is not None:
                    deps.add(st.w)
        for x in writes:
            t, k = norm(x)
            for st in t.states(k):
                if st.w is not None:
                    deps.add(st.w)
                for e, oid in st.r.items():
                    deps.add(oid)
        for x in reads:
            t, k = norm(x)
            t.target(k).r[key_e] = o.id
        for x in writes:
            t, k = norm(x)
            if k is None:
                for st in t.states(None):
                    st.w = o.id
                    st.r = {}
            else:
                st = t.target(k)
                st.w = o.id
                st.r = {}
        waits = {}
        for d in deps:
            po = self.ops[d]
            if (not po.dma) and (not dma) and po.eng == eng and eng == "pe":
                continue
            assert po.sem is not None, "dependency on non-signalling op (%s)" % po.eng
            key = id(po.sem)
            if key not in waits or waits[key][1] < po.semval:
                waits[key] = (po.sem, po.semval)
        E = self.engobj[eng]
        known = self.known[eng]
        if dma:
            n = self.dcnt[eng]
            self.dcnt[eng] += 1
            o.sem = self.dsem[eng][n % self.ndma]
            o.semval = 16 * (n // self.ndma + 1)
            if n >= self.ndma:
                key = id(o.sem)
                v = 16 * (n // self.ndma)
                if key not in waits or waits[key][1] < v:
                    waits[key] = (o.sem, v)
        for key, (s, v) in waits.items():
            if known.get(key, 0) >= v:
                continue
            E.wait_ge(s, v)
            self.n_waits += 1
            known[key] = v
        ins = fn(E)
        if dma:
            ins.then_inc(o.sem, 16)
        elif o.sig:
            self.cnt[eng] += 1
            o.sem = self.esem[eng]
            o.semval = self.cnt[eng]
            ins.then_inc(o.sem, 1)
            if eng == "pe":
                for q in self.pending_pe:
                    q.sem, q.semval = o.sem, o.semval
                self.pending_pe = []
        elif eng == "pe":
            self.pending_pe.append(o)
        self.ops.append(o)
        return o

    def emit(self):
        pass


def _t5_bucket_np(rel):
    nb = 16
    max_exact = 8
    ret = (rel > 0).astype(np.int32) * nb
    n = np.abs(rel)
    large = max_exact + (np.log(np.maximum(n, 1).astype(np.float32) / max_exact)
                         / math.log(128 / max_exact) * (nb - max_exact)).astype(np.int32)
    large = np.minimum(large, nb - 1)
    return ret + np.where(n < max_exact, n, large)


def _host_tables():
    f32 = np.float32
    heads = np.arange(4, dtype=f32)
    lgf = np.log1p(-np.exp2(-5.0 - heads)).astype(f32)
    lgb = np.log1p(-np.exp2(-5.5 - heads)).astype(f32)
    C = 128
    idx = np.arange(C, dtype=f32)
    inv = (1.0 / (10000.0 ** np.linspace(0.0, 1.0, 64, dtype=f32))).astype(f32)
    pos = np.arange(S_LEN, dtype=f32)
    ang = pos[:, None] * inv[None, :]
    cos = np.cos(ang).astype(f32).reshape(NT, 128, 64).transpose(1, 0, 2)
    sin = np.sin(ang).astype(f32).reshape(NT, 128, 64).transpose(1, 0, 2)
    ksc = f32(128 ** -0.5)
    cosT = np.ascontiguousarray(cos)
    sinT = np.ascontiguousarray(sin)
    diff = idx[:, None] - idx[None, :]
    maskT = np.zeros((4, C, C), f32)
    for h in range(4):
        Mf = np.where(diff >= 0, np.exp(np.maximum(diff, 0.0) * lgf[h]), 0.0)
        Mb = np.where(diff < 0, np.exp(np.maximum(-diff, 0.0) * lgb[h]), 0.0)
        maskT[h] = ((Mf + Mb).astype(f32) * ksc).T
    maskT = np.ascontiguousarray(maskT.transpose(1, 0, 2))
    kdec = np.zeros((C, 8), f32)
    qdec = np.zeros((8, C), f32)
    for h in range(4):
        kdec[:, h] = np.exp((C - 1 - idx) * lgf[h]) * ksc
        kdec[:, 4 + h] = np.exp(idx * lgb[h]) * ksc
        qdec[h] = np.exp((idx + 1) * lgf[h])
        qdec[4 + h] = np.exp((C - idx) * lgb[h])
    qdec_bc = np.ascontiguousarray(np.broadcast_to(qdec.reshape(1, 8 * C), (128, 8 * C))).astype(f32)
    cdec = [float(np.exp(f32(C) * lgf[h])) for h in range(4)] + [float(np.exp(f32(C) * lgb[h])) for h in range(4)]
    ident = np.eye(128, dtype=f32)
    kl = np.arange(128)[:, None, None]
    j = np.arange(3)[None, :, None]
    ql = np.arange(128)[None, None, :]
    bidx = _t5_bucket_np(kl - ql - (j - 1) * 128)
    iota = np.ascontiguousarray(np.broadcast_to(np.arange(256, dtype=f32)[None, :], (128, 256)))
    pt = np.zeros((128, NT, 2), f32)
    pt[:, :, 0] = np.arange(128)[:, None]
    pt[:, :, 1] = np.arange(NT)[None, :]
    return dict(cosT=cosT, sinT=sinT, maskT=maskT, kdec=kdec, qdec=qdec_bc, cdec=cdec, ident=ident,
                bidx=bidx, iota=iota, pt=pt)


def build_program(tabs, stop_after=None, dbg=None):
    nc = bass.Bass("TRN2", target_bir_lowering=False)
    S = Sched(nc)
    dbg = dbg or []
    dbg_out = {}
    ins = {}

    def din(name, shape, dt=F32):
        t = Tile(nc.dram_tensor(name, list(shape), dt, kind="ExternalInput").ap(), name)
        ins[name] = t
        return t

    x_d = din("x", [S_LEN, D])
    cT_d = din("cT", [128, 8])
    wada_d = din("w_ada", [D, 6 * D])
    bada_d = din("b_ada", [1, 6 * D])
    g1T_d = din("g1T", [128, 8])
    g2T_d = din("g2T", [128, 8])
    fing_d = din("final_g", [1, D])
    win_d = din("w_in", [D, 5632])
    gn_d = din("ret_gn_g", [1, 512])
    subln_d = din("subln_g", [1, 128])
    lam_d = din("lam4", [1, 256])
    wro_d = din("w_ret_out", [512, D])
    wdo_d = din("w_diff_out", [512, D])
    wo_d = din("w_o", [D, D])
    biasT_d = din("biasT", [128, 4, 384])
    bfar_d = din("bfar", [128, 8])
    wr_d = din("w_router", [D, NE])
    wg_d = din("w_gate", [NE, D, D])
    wu_d = din("w_up", [NE, D, D])
    wd_d = din("w_down", [NE, D, D])
    cos_d = din("cosT", [128, NT, 64])
    sin_d = din("sinT", [128, NT, 64])
    maskT_d = din("maskT", [128, 4, 128])
    kdec_d = din("kdec", [128, 8])
    qdec_d = din("qdec", [128, 8 * 128])
    ident_d = din("ident", [128, 128])
    iota_d = din("iota", [128, 256])
    pt_d = din("pt", [128, NT, 2])
    out_d = Tile(nc.dram_tensor("out", [S_LEN, D], F32, kind="ExternalOutput").ap(), "out")
    acc_d = Tile(nc.dram_tensor("acc_scr", [S_LEN, D], F32, kind="Internal").ap(), "acc")
    xn2_d = Tile(nc.dram_tensor("xn2_scr", [S_LEN, D], BF16, kind="Internal").ap(), "xn2")
    cdec = tabs["cdec"]
    wb_d = [Tile(nc.dram_tensor("wb_scr%d" % a, [NE, 128, 8 * D], BF16, kind="Internal").ap(), "wb%d" % a) for a in range(3)]
    wsrc_f = (wg_d, wu_d, wd_d)
    conv_i = [0]

    def conv_batch(n):
        for _ in range(n):
            i = conv_i[0]
            if i >= 3 * NE:
                return
            conv_i[0] += 1
            e_, a = i // 3, i % 3
            S.op("pool", lambda e, e_=e_, a=a: e.dma_start(
                out=wb_d[a].h[e_].rearrange("p (k n) -> p k n", k=8),
                in_=wsrc_f[a].h[e_].rearrange("(k p) n -> p k n", p=128)), reads=[wsrc_f[a]], writes=[(wb_d[a], e_)],
                dma=True)
    final_reads = [out_d]

    def debug_dump(name, tile, shape, dt=F32, src=None):
        if name not in dbg:
            return
        t = Tile(nc.dram_tensor("dbg_" + name, list(shape), dt, kind="ExternalOutput").ap(), name)
        dbg_out[name] = t
        S.op("sp", lambda e: e.dma_start(out=t.h, in_=(src if src is not None else tile[:])), reads=[tile], writes=[t], dma=True)
        final_reads.append(t)

    def load(eng, dst, dst_ap, src, src_ap, wkey=None):
        S.op(eng, lambda e: e.dma_start(out=dst_ap, in_=src_ap), reads=[src],
             writes=[(dst, wkey) if wkey is not None else dst], dma=True)

    def pview(bank, dt, a, b):
        ap = bank[:, :]
        if dt == BF16:
            ap = ap.bitcast(BF16)
        return ap[:, 0:a * b].rearrange("p (a b) -> p a b", a=a)

    def bc3(ap2, n):
        return ap2.unsqueeze(2).to_broadcast([ap2.shape[0], ap2.shape[1], n])

    def bcm(ap2, n):
        return ap2.unsqueeze(1).to_broadcast([ap2.shape[0], n, ap2.shape[1]])

    ident_f = S.sb("ident_f", [128, 128], F32)
    ident_b = S.sb("ident_b", [128, 128], BF16)
    ones_b = S.sb("ones_b", [128, 128], BF16)
    load("sp", ident_f, ident_f[:], ident_d, ident_d.h)
    S.op("dve", lambda e: e.tensor_copy(out=ident_b[:], in_=ident_f[:]), reads=[ident_f], writes=[ident_b])
    S.op("pool", lambda e: e.memset(ones_b[:], 1.0), writes=[ones_b])
    eps_g = S.sb("eps_g", [128, 1], F32)
    S.op("pool", lambda e: e.memset(eps_g[:], EPS), writes=[eps_g])
    modT = S.sb("modT", [128, 4, 8], F32)
    gscT = S.sb("gscT", [128, 2, 8], F32)
    ga_bc = S.sb("ga_bc", [128, 2, D], F32)
    hT = S.sb("hT", [128, 8, S_LEN], BF16)
    aff = S.sb("aff", [128, NT, NE], F32)
    posTM = S.sb("posTM", [128, NT, NE], BF16)
    idx_c = [S.sb("idx_c%d" % i, [128, 1], I32) for i in range(2 * NE)]
    gate = S.sb("gate", [128, 2 * NE], F32)
    iota_b = S.sb("iota_b", [128, 256], BF16)
    fing_bc = S.sb("fing_bc", [128, D], F32)
    load("sp", fing_bc, fing_bc[:], fing_d, fing_d.h.partition_broadcast(128))
    S.push()
    yT = S.sb("yT", [128, 8, S_LEN], BF16)
    ring = [S.sb("ring%d" % i, [128, 4096], BF16) for i in range(4)]
    ring_i = [0]

    def slot():
        r = ring[ring_i[0] % len(ring)]
        ring_i[0] += 1
        return r

    def norm_stats(src_tile, t, ss, rstd, xn_bufs, xn_dst=None):
        junk = xn_bufs["junk"]
        S.op("act", lambda e: e.activation(out=junk[:], in_=src_tile[:], func=AF.Square, accum_out=ss[:, t:t + 1]),
             reads=[src_tile], writes=[junk, (ss, t)])
        S.op("act", lambda e: e.activation(out=ss[:, t:t + 1], in_=ss[:, t:t + 1], func=AF.Ln, scale=1.0 / D,
                                           bias=eps_g[:, 0:1]), reads=[(ss, t), eps_g], writes=[(ss, t)])
        S.op("act", lambda e: e.activation(out=rstd[:, t:t + 1], in_=ss[:, t:t + 1], func=AF.Exp, scale=-0.5),
             reads=[(ss, t)], writes=[(rstd, t)])
        if xn_dst is not None:
            xtile, xkey, xap = xn_dst
            S.op("act", lambda e: e.activation(out=xap, in_=src_tile[:], func=AF.Copy, scale=rstd[:, t:t + 1]),
                 reads=[src_tile, (rstd, t)], writes=[(xtile, xkey)])
            return None
        xn = xn_bufs["xn"][t % 2]
        S.op("act", lambda e: e.activation(out=xn[:], in_=src_tile[:], func=AF.Copy, scale=rstd[:, t:t + 1]),
             reads=[src_tile, (rstd, t)], writes=[xn])
        return xn

    def transp_evac(xn, t, which, dstT, pT_bufs, tmpT, xn_src=None, act_evac=True):
        pTk = pT_bufs[t % 2]
        pT = pview(pTk, BF16, 8, 128)
        if xn_src is not None:
            xtile, xkey, xap = xn_src
            rd = (xtile, xkey)
        else:
            xap = xn[:]
            rd = xn
        for j in range(8):
            S.op("pe", lambda e, j=j: e.transpose(out=pT[:, j, :], in_=xap[:, j * 128:(j + 1) * 128],
                                                  identity=ident_b[:]), reads=[rd, ident_b], writes=[pTk],
                 sig=(j == 7))
        if t % 2 == 1 and act_evac:
            for j in range(8):
                S.op("act", lambda e, j=j: e.activation(out=dstT[:, j, t * 128:(t + 1) * 128], in_=pT[:, j, :],
                                                        func=AF.Identity, scale=gscT[:, which, j:j + 1],
                                                        bias=modT[:, 2 * which, j:j + 1]),
                     reads=[pTk, gscT, modT], writes=[(dstT, t)])
            return
        S.op("dve", lambda e: e.tensor_tensor(out=tmpT[:], in0=pT, in1=bc3(gscT[:, which, :], 128), op=ALU.mult),
             reads=[pTk, gscT], writes=[tmpT])
        S.op("dve", lambda e: e.tensor_tensor(out=dstT[:, :, t * 128:(t + 1) * 128], in0=tmpT[:],
                                              in1=bc3(modT[:, 2 * which, :], 128), op=ALU.add),
             reads=[tmpT, modT], writes=[(dstT, t)])


    S.push()
    xt = [S.sb("xt%d" % i, [128, D], F32) for i in range(3)]
    nb = dict(junk=S.sb("junk", [128, D], BF16))
    xn_all = S.sb("xn_all", [128, NT, D], BF16)
    ssB = S.sb("ssB", [128, NT], F32)
    rstdB = S.sb("rstdB", [128, NT], F32)
    tmpT = S.sb("tmpT", [128, 8, 128], F32)
    pTb = [S.ps("pTb%d" % i, [128, 512], F32) for i in range(2)]
    cT = S.sb("cT_sb", [128, 8], F32)
    scT = S.sb("scT", [128, 8], F32)
    crep = S.sb("crep", [128, 8, 128], BF16)
    b_blk = [S.sb("b_blk%d" % i, [128, 512], F32) for i in range(3)]
    mod_tmp = S.sb("mod_tmp", [128, 4, D], F32)
    g12T = S.sb("g12T", [128, 2, 8], F32)
    dtmp = S.sb("dtmp", [128, 8, 128], F32)
    load("sp", cT, cT[:], cT_d, cT_d.h)
    load("sp", g12T, g12T[:, 0, :], g1T_d, g1T_d.h)
    load("sp", g12T, g12T[:, 1, :], g2T_d, g2T_d.h)
    S.op("act", lambda e: e.activation(out=scT[:], in_=cT[:], func=AF.Silu), reads=[cT], writes=[scT])
    for k in range(8):
        S.op("dve", lambda e, k=k: e.tensor_scalar(out=crep[:, k, :], in0=ones_b[:], scalar1=scT[:, k:k + 1],
                                                  scalar2=None, op0=ALU.mult), reads=[ones_b, scT], writes=[crep])
    for t in range(NT):
        xb = xt[t % 3]
        load("sp", xb, xb[:], x_d, x_d[t * 128:(t + 1) * 128, :])
        norm_stats(xb, t, ssB, rstdB, nb, xn_dst=(xn_all, t, xn_all[:, t, :]))
    wada_v = wada_d.h.rearrange("(k p) n -> p k n", p=128)
    pA = [S.ps("pA%d" % i, [128, 512], F32) for i in range(2)]
    seg_dst = {0: ("m", 0), 1: ("m", 1), 2: ("g", 0), 3: ("m", 2), 4: ("m", 3), 5: ("g", 1)}
    def extract_mod(si):
        S.op("dve", lambda e: e.tensor_tensor(
            out=dtmp[:], in0=mod_tmp[:, si, :].rearrange("p (a b) -> p a b", a=8),
            in1=bcm(ident_f[:], 8), op=ALU.mult), reads=[(mod_tmp, si), ident_f], writes=[dtmp])
        S.op("dve", lambda e: e.tensor_reduce(out=modT[:, si, :], in_=dtmp[:], axis=AX.X, op=ALU.add),
             reads=[dtmp], writes=[modT])

    def mk_gsc(i, si):
        S.op("dve", lambda e: e.scalar_tensor_tensor(
            out=gscT[:, i, :], in0=modT[:, si, :], scalar=1.0, in1=g12T[:, i, :], op0=ALU.add, op1=ALU.mult),
            reads=[modT, g12T], writes=[gscT])

    for blk in range(12):
        w = slot()
        wv = w[:, :].rearrange("p (k n) -> p k n", k=8)
        load("pool", w, wv, wada_d, wada_v[:, :, blk * 512:(blk + 1) * 512])
        p = pA[blk % 2]
        for k in range(8):
            S.op("pe", lambda e, p=p, k=k, wv=wv: e.matmul(
                p[:], lhsT=crep[:, k, :], rhs=wv[:, k, :], start=(k == 0), stop=(k == 7)),
                reads=[crep, w], writes=[p], sig=(k == 7))
        kind, si = seg_dst[blk // 2]
        half = blk % 2
        dst = (mod_tmp[:, si, half * 512:(half + 1) * 512] if kind == "m"
               else ga_bc[:, si, half * 512:(half + 1) * 512])
        dt_ = (mod_tmp, si) if kind == "m" else ga_bc
        c0 = blk * 512
        bb = b_blk[blk % 3]
        load("sp", bb, bb[:], bada_d, bada_d.h[:, c0:c0 + 512].partition_broadcast(128))
        S.op("dve", lambda e, p=p, dst=dst, bb=bb: e.tensor_tensor(out=dst, in0=p[:], in1=bb[:], op=ALU.add),
             reads=[p, bb], writes=[dt_])
        if blk == 3:
            extract_mod(0)
            extract_mod(1)
            mk_gsc(0, 1)
            for t in range(NT):
                transp_evac(None, t, 0, hT, pTb, tmpT, xn_src=(xn_all, t, xn_all[:, t, :]))
    extract_mod(2)
    extract_mod(3)
    mk_gsc(1, 3)
    debug_dump("modT", modT, [128, 4, 8])
    debug_dump("ga_bc", ga_bc, [128, 2, D])
    debug_dump("hT", hT, [128, 8, S_LEN], BF16)
    S.pop()
    if stop_after in ("A", "B"):
        S.pop()
        return finish(nc, S, ins, dbg_out, final_reads)

    win_v = win_d.h.rearrange("(k p) n -> p k n", p=128)

    S.push()
    cos_sb = S.sb("cos_sb", [128, NT, 64], F32)
    sin_sb = S.sb("sin_sb", [128, NT, 64], F32)
    maskT_sb = S.sb("maskT_sb", [128, 4, 128], F32)
    kdec_sb = S.sb("kdec_sb", [128, 8], F32)
    qdec_sb = S.sb("qdec_sb", [128, 8, 128], F32)
    gn_bc = S.sb("gn_bc", [128, 512], F32)
    load("sp", cos_sb, cos_sb[:], cos_d, cos_d.h)
    load("sp", sin_sb, sin_sb[:], sin_d, sin_d.h)
    load("sp", maskT_sb, maskT_sb[:], maskT_d, maskT_d.h)
    load("sp", kdec_sb, kdec_sb[:], kdec_d, kdec_d.h)
    load("sp", qdec_sb, qdec_sb[:].rearrange("p a b -> p (a b)"), qdec_d, qdec_d.h)
    load("sp", gn_bc, gn_bc[:], gn_d, gn_d.h.partition_broadcast(128))
    qkTM = S.sb("qkTM", [128, NT, 2, 128], BF16)
    vTM = S.sb("vTM", [128, NT, 128], BF16)
    vFB = S.sb("vFB", [128, NT, 2, 128], BF16)
    rgs = S.sb("rgs", [128, NT, 128], BF16)
    qkT = S.sb("qkT", [128, 2, S_LEN], BF16)
    qTfb = S.sb("qTfb", [128, 2, S_LEN], BF16)
    prevFB = S.sb("prevFB", [128, 2, NT, 128], BF16)
    stFB = [S.sb("stFB%d" % i, [128, 128], F32) for i in range(2)]
    yr = S.sb("yr", [128, NT, 128], F32)
    yc = S.sb("yc", [128, NT, 128], F32)
    sqj = S.sb("sqj", [128, 128], BF16)
    yret = S.sb("yret", [128, NT, 128], BF16)
    rtc = [S.sb("rtc%d" % i, [128, 2, 64, 2], F32) for i in range(2)]
    rts = [S.sb("rts%d" % i, [128, 2, 64, 2], F32) for i in range(2)]
    qkraw = [S.sb("qkraw%d" % i, [128, 256], F32) for i in range(2)]
    sT = [S.sb("sT%d" % i, [128, 128], BF16) for i in range(2)]
    st1 = S.sb("st1", [128, NT], F32)
    st2 = S.sb("st2", [128, NT], F32)
    st3 = S.sb("st3", [128, NT], F32)
    pr = [S.ps("pr%d" % i, [128, 512], F32) for i in range(2)]
    pq = [S.ps("pq%d" % i, [128, 512], F32) for i in range(2)]
    pkv = [S.ps("pkv0", [128, 512], F32)]
    pssb = [S.ps("pss%d" % i, [128, 512], F32) for i in range(2)]
    po = S.ps("po", [128, 512], F32)
    def load_R(h):
        w = slot()
        wv = w[:, :].rearrange("p (k s c) -> p k s c", k=8, s=4)
        for s_ in range(4):
            c0 = s_ * 512 + h * 128
            load("pool", w, wv[:, :, s_, :], win_d, win_v[:, :, c0:c0 + 128])
        return w

    def load_D(h):
        w = slot()
        wq = w[:, 0:1024].rearrange("p (k n) -> p k n", k=8)
        wk = w[:, 1024:2048].rearrange("p (k n) -> p k n", k=8)
        wvv = w[:, 2048:3072].rearrange("p (k n) -> p k n", k=8)
        load("pool", w, wq[:, :, 0:64], win_d, win_v[:, :, 2048 + h * 64:2048 + (h + 1) * 64])
        load("pool", w, wq[:, :, 64:128], win_d, win_v[:, :, 2304 + h * 64:2304 + (h + 1) * 64])
        load("pool", w, wk[:, :, 0:64], win_d, win_v[:, :, 2560 + h * 64:2560 + (h + 1) * 64])
        load("pool", w, wk[:, :, 64:128], win_d, win_v[:, :, 2816 + h * 64:2816 + (h + 1) * 64])
        load("pool", w, wvv, win_d, win_v[:, :, 3072 + h * 128:3072 + (h + 1) * 128])
        return w

    wnext = load_R(0)
    for h in range(4):
        w = wnext
        wnext = load_R(h + 1) if h < 3 else load_D(0)
        conv_batch(6)
        wflat = w[:, :].rearrange("p (k n) -> p k n", k=8)

        def r_proj(t):
            p = pr[t % 2]
            for k in range(8):
                S.op("pe", lambda e, k=k: e.matmul(
                    p[:], lhsT=hT[:, k, t * 128:(t + 1) * 128], rhs=wflat[:, k, :], start=(k == 0), stop=(k == 7)),
                    reads=[(hT, t), w], writes=[p], sig=(k == 7))

        def r_evac(t):
            p = pr[t % 2]
            raw = qkraw[t % 2]
            S.op("act", lambda e: e.activation(out=raw[:], in_=p[:, 0:256], func=AF.Copy), reads=[p], writes=[raw])
            S.op("act", lambda e: e.activation(out=vTM[:, t, :], in_=p[:, 256:384], func=AF.Copy), reads=[p],
                 writes=[(vTM, t)])
            S.op("act", lambda e: e.activation(out=rgs[:, t, :], in_=p[:, 384:512], func=AF.Silu), reads=[p],
                 writes=[(rgs, t)])
            S.op("pool", lambda e: e.tensor_tensor(out=rgs[:, t, :], in0=rgs[:, t, :], in1=gn_bc[:, h * 128:(h + 1) * 128],
                                                   op=ALU.mult), reads=[(rgs, t), gn_bc], writes=[(rgs, t)])
            for dr in range(2):
                S.op("dve", lambda e, dr=dr: e.tensor_scalar(
                    out=vFB[:, t, dr, :], in0=p[:, 256:384], scalar1=kdec_sb[:, 4 * dr + h:4 * dr + h + 1],
                    scalar2=None, op0=ALU.mult), reads=[p, kdec_sb], writes=[(vFB, t)])
            rv = raw[:].rearrange("p (a i two) -> p a i two", a=2, two=2)
            cv = cos_sb[:, t, :].unsqueeze(1).unsqueeze(3).to_broadcast([128, 2, 64, 2])
            sv = sin_sb[:, t, :].unsqueeze(1).unsqueeze(3).to_broadcast([128, 2, 64, 2])
            tc_ = rtc[t % 2]
            ts_ = rts[t % 2]
            ov = qkTM[:, t, :, :].rearrange("p a (i two) -> p a i two", two=2)
            S.op("pool", lambda e: e.tensor_tensor(out=tc_[:], in0=rv, in1=cv, op=ALU.mult), reads=[raw, cos_sb],
                 writes=[tc_])
            S.op("dve", lambda e: e.tensor_tensor(out=ts_[:], in0=rv, in1=sv, op=ALU.mult), reads=[raw, sin_sb],
                 writes=[ts_])
            S.op("pool", lambda e: e.tensor_tensor(out=ov[:, :, :, 0], in0=tc_[:, :, :, 0], in1=ts_[:, :, :, 1],
                                                   op=ALU.subtract), reads=[tc_, ts_], writes=[(qkTM, (t, 0))])
            S.op("dve", lambda e: e.tensor_tensor(out=ov[:, :, :, 1], in0=tc_[:, :, :, 1], in1=ts_[:, :, :, 0],
                                                  op=ALU.add), reads=[tc_, ts_], writes=[(qkTM, (t, 1))])

        def r_T(t):
            pqk = pq[t % 2]
            pqt = pview(pqk, BF16, 2, 128)
            for a in range(2):
                S.op("pe", lambda e, a=a: e.transpose(out=pqt[:, a, :], in_=qkTM[:, t, a, :], identity=ident_b[:]),
                     reads=[(qkTM, (t, 0)), (qkTM, (t, 1)), ident_b], writes=[pqk], sig=(a == 1))

        def r_F(t):
            pqk = pq[t % 2]
            pqt = pview(pqk, BF16, 2, 128)
            S.op("act", lambda e: e.activation(out=qkT[:, :, t * 128:(t + 1) * 128], in_=pqt, func=AF.Copy),
                 reads=[pqk], writes=[(qkT, t)])
            for dr in range(2):
                S.op("dve", lambda e, dr=dr: e.tensor_tensor(
                    out=qTfb[:, dr, t * 128:(t + 1) * 128], in0=pqt[:, 0, :], in1=qdec_sb[:, 4 * dr + h, :],
                    op=ALU.mult), reads=[pqk, qdec_sb], writes=[(qTfb, t)])

        r_proj(0)
        r_evac(0)
        r_proj(1)
        for t in range(NT):
            if t + 1 < NT:
                r_evac(t + 1)
            r_T(t)
            if t + 2 < NT:
                r_proj(t + 2)
            r_F(t)
        if stop_after == 'R0a':
            debug_dump('qkTM0', qkTM, [128, NT, 2, 128], BF16)
            S.pop()
            return finish(nc, S, ins, dbg_out, final_reads)
        for dr in range(2):
            S.op("pool", lambda e, dr=dr: e.memset(stFB[dr][:], 0.0), writes=[stFB[dr]])
        kvb = [pkv[0], pq[0], pq[1]]
        kvi = 0
        orders = [list(range(NT)), list(range(NT - 1, -1, -1))]
        for i in range(NT):
            for dr in range(2):
                n = orders[dr][i]
                S.op("dve" if dr == 0 else "pool", lambda e, dr=dr, n=n: e.tensor_copy(out=prevFB[:, dr, n, :],
                                                                                     in_=stFB[dr][:]),
                     reads=[stFB[dr]], writes=[(prevFB, (dr, n))])
                if i == NT - 1:
                    continue
                pk = kvb[kvi % 3]
                kvi += 1
                S.op("pe", lambda e, pk=pk, n=n, dr=dr: e.matmul(pk[:, 0:128], lhsT=qkTM[:, n, 1, :],
                                                                 rhs=vFB[:, n, dr, :], start=True, stop=True),
                     reads=[(qkTM, (n, 0)), (qkTM, (n, 1)), (vFB, n)], writes=[pk])
                S.op("dve", lambda e, pk=pk, dr=dr: e.scalar_tensor_tensor(
                    out=stFB[dr][:], in0=stFB[dr][:], scalar=cdec[4 * dr + h], in1=pk[:, 0:128], op0=ALU.mult,
                    op1=ALU.add), reads=[stFB[dr], pk], writes=[stFB[dr]])
        if stop_after == 'R0b':
            debug_dump('prevFB', prevFB, [128, 2, NT, 128], BF16)
            S.pop()
            return finish(nc, S, ins, dbg_out, final_reads)
        def c_score(n):
            cs = slice(n * 128, (n + 1) * 128)
            pss = pssb[n % 2]
            S.op("pe", lambda e: e.matmul(pss[:, 0:128], lhsT=qkT[:, 1, cs], rhs=qkT[:, 0, cs], start=True, stop=True),
                 reads=[(qkT, n)], writes=[pss])

        def c_rest(n, nxt):
            cs = slice(n * 128, (n + 1) * 128)
            pss = pssb[n % 2]
            sTb = sT[n % 2]
            S.op("dve", lambda e: e.tensor_tensor(out=sTb[:], in0=pss[:, 0:128], in1=maskT_sb[:, h, :], op=ALU.mult),
                 reads=[pss, maskT_sb], writes=[sTb])
            if nxt is not None:
                c_score(nxt)
            S.op("pe", lambda e: e.matmul(po[:, 0:128], lhsT=sTb[:], rhs=vTM[:, n, :], start=True, stop=False),
                 reads=[sTb, (vTM, n)], writes=[po], sig=False)
            S.op("pe", lambda e: e.matmul(po[:, 0:128], lhsT=qTfb[:, 0, cs], rhs=prevFB[:, 0, n, :], start=False,
                                          stop=False), reads=[(qTfb, n), (prevFB, (0, n))], writes=[po], sig=False)
            S.op("pe", lambda e: e.matmul(po[:, 0:128], lhsT=qTfb[:, 1, cs], rhs=prevFB[:, 1, n, :], start=False,
                                          stop=True), reads=[(qTfb, n), (prevFB, (1, n))], writes=[po])
            S.op("act", lambda e: e.activation(out=yr[:, n, :], in_=po[:, 0:128], func=AF.Copy,
                                               accum_out=st1[:, n:n + 1]), reads=[po], writes=[(yr, n), (st1, n)])
            S.op("act", lambda e: e.activation(out=sqj[:], in_=yr[:, n, :], func=AF.Square, accum_out=st2[:, n:n + 1]),
                 reads=[(yr, n)], writes=[sqj, (st2, n)])

        c_score(0)
        for n in range(NT):
            c_rest(n, n + 1 if n + 1 < NT else None)
        if h == 0:
            debug_dump("yr0", yr, [128, NT, 128])
            debug_dump("qkTM0", qkTM, [128, NT, 2, 128], BF16)
        if stop_after == 'R0c':
            S.pop()
            return finish(nc, S, ins, dbg_out, final_reads)
        S.op("dve", lambda e: e.tensor_scalar(out=st1[:], in0=st1[:], scalar1=1.0 / 128, scalar2=None, op0=ALU.mult),
             reads=[st1], writes=[st1])
        S.op("dve", lambda e: e.tensor_tensor(out=st3[:], in0=st1[:], in1=st1[:], op=ALU.mult), reads=[st1],
             writes=[st3])
        S.op("dve", lambda e: e.scalar_tensor_tensor(out=st2[:], in0=st2[:], scalar=1.0 / 128, in1=st3[:],
                                                     op0=ALU.mult, op1=ALU.subtract), reads=[st2, st3], writes=[st2])
        S.op("act", lambda e: e.activation(out=st2[:], in_=st2[:], func=AF.Ln, bias=eps_g[:, 0:1]), reads=[st2, eps_g],
             writes=[st2])
        S.op("act", lambda e: e.activation(out=st3[:], in_=st2[:], func=AF.Exp, scale=-0.5), reads=[st2], writes=[st3])
        for n in range(NT):
            S.op("dve", lambda e, n=n: e.tensor_scalar(out=yc[:, n, :], in0=yr[:, n, :], scalar1=st1[:, n:n + 1],
                                                      scalar2=st3[:, n:n + 1], op0=ALU.subtract, op1=ALU.mult),
                 reads=[(yr, n), st1, st3], writes=[(yc, n)])
        S.op("dve", lambda e: e.tensor_tensor(out=yret[:], in0=yc[:], in1=rgs[:], op=ALU.mult), reads=[yc, rgs],
             writes=[yret])
        for g in range(4):
            pt4 = pr[g % 2]
            ptv = pview(pt4, BF16, 4, 128)
            for j in range(4):
                S.op("pe", lambda e, ptv=ptv, j=j, g=g: e.transpose(out=ptv[:, j, :], in_=yret[:, 4 * g + j, :],
                                                                   identity=ident_b[:]),
                     reads=[yret, ident_b], writes=[pt4])
            S.op("act", lambda e, ptv=ptv, g=g, h=h: e.activation(
                out=yT[:, h, g * 512:(g + 1) * 512].rearrange("p (a b) -> p a b", a=4), in_=ptv, func=AF.Copy),
                reads=[pt4], writes=[(yT, (h, g))])
    debug_dump("yT", yT, [128, 8, S_LEN], BF16)
    S.pop()
    if stop_after == "R":
        S.pop()
        return finish(nc, S, ins, dbg_out, final_reads)

    S.push()
    LAM_INIT = 0.8 - 0.6 * math.exp(-0.3 * 0)
    biasT_sb = S.sb("biasT_sb", [128, 4, 384], F32)
    bfar_sb = S.sb("bfar_sb", [128, 8], F32)
    lam_bc = S.sb("lam_bc", [128, 4, 64], F32)
    lam_t = S.sb("lam_t", [128, 2, 64], F32)
    lam_s = S.sb("lam_s", [128, 4], F32)
    neglam = S.sb("neglam", [128, 1], F32)
    subln_bc = S.sb("subln_bc", [128, 128], F32)
    load("sp", biasT_sb, biasT_sb[:], biasT_d, biasT_d.h)
    load("sp", bfar_sb, bfar_sb[:], bfar_d, bfar_d.h)
    load("sp", lam_bc, lam_bc[:].rearrange("p a b -> p (a b)"), lam_d, lam_d.h.partition_broadcast(128))
    load("sp", subln_bc, subln_bc[:], subln_d, subln_d.h.partition_broadcast(128))
    S.op("dve", lambda e: e.tensor_scalar(out=subln_bc[:], in0=subln_bc[:], scalar1=1.0 - LAM_INIT, scalar2=None,
                                          op0=ALU.mult), reads=[subln_bc], writes=[subln_bc])
    for i in range(2):
        S.op("dve", lambda e, i=i: e.tensor_tensor(out=lam_t[:, i, :], in0=lam_bc[:, 2 * i, :], in1=lam_bc[:, 2 * i + 1, :],
                                                  op=ALU.mult), reads=[lam_bc], writes=[lam_t])
    S.op("dve", lambda e: e.tensor_reduce(out=lam_s[:, 0:2], in_=lam_t[:], axis=AX.X, op=ALU.add), reads=[lam_t],
         writes=[lam_s])
    S.op("act", lambda e: e.activation(out=lam_s[:, 2:4], in_=lam_s[:, 0:2], func=AF.Exp), reads=[lam_s], writes=[lam_s])
    S.op("dve", lambda e: e.tensor_tensor(out=neglam[:], in0=lam_s[:, 3:4], in1=lam_s[:, 2:3], op=ALU.subtract),
         reads=[lam_s], writes=[neglam])
    S.op("dve", lambda e: e.tensor_scalar(out=neglam[:], in0=neglam[:], scalar1=-LAM_INIT, scalar2=None, op0=ALU.add),
         reads=[neglam], writes=[neglam])
    qT_d = S.sb("qT_d", [128, S_LEN], BF16)
    kT_d = S.sb("kT_d", [128, 2, S_LEN], BF16)
    S.op("pool", lambda e: e.memset(kT_d[:], 0.0), writes=[kT_d])
    v_aug = S.sb("v_aug", [128, NT, 130], BF16)
    pT_sb = [[S.sb("pT_sb%d%d" % (m, i), [128, 512], BF16) for i in range(2)] for m in range(2)]
    btmp = [S.sb("btmp%d" % i, [128, 384], F32) for i in range(2)]
    osb = [S.sb("osb%d" % m, [128, 512], F32) for m in range(2)]
    recb = [S.sb("recb%d" % m, [128, 512], F32) for m in range(2)]
    ydT = S.sb("ydT", [128, 512], F32)
    ysqT = S.sb("ysqT", [128, 512], F32)
    rstdT = S.sb("rstdT", [128, 512], F32)
    ones_f = S.sb("ones_f", [128, 128], F32)
    sublnT = S.sb("sublnT", [128, 1], F32)
    S.op("pool", lambda e: e.memset(ones_f[:], 1.0), writes=[ones_f])
    eps_c = S.sb("eps_c", [128, 1], F32)
    S.op("pool", lambda e: e.memset(eps_c[:], EPS), writes=[eps_c])
    with nc.allow_non_contiguous_dma(reason="tiny 128-element column load"):
        load("sp", sublnT, sublnT[:], subln_d, subln_d.h.rearrange("o d -> d o"))
    S.op("dve", lambda e: e.tensor_scalar(out=sublnT[:], in0=sublnT[:], scalar1=1.0 - LAM_INIT, scalar2=None,
                                          op0=ALU.mult), reads=[sublnT], writes=[sublnT])
    scb = [S.ps("scb%d" % m, [128, 512], F32) for m in range(3)]
    Ob = [S.ps("Ob%d" % i, [128, 512], F32) for i in range(2)]
    Rb = [S.ps("Rb%d" % i, [128, 512], F32) for i in range(2)]
    pm = S.ps("pm", [128, 512], F32)
    prb = pm

    btc = [0]
    wGpre = None
    pend_norm = []
    for h in range(4):
        w = wnext
        wq = w[:, 0:1024].rearrange("p (k n) -> p k n", k=8)
        wk = w[:, 1024:2048].rearrange("p (k n) -> p k n", k=8)
        wvv = w[:, 2048:3072].rearrange("p (k n) -> p k n", k=8)
        if h < 3:
            wnext = load_D(h + 1)
        else:
            wRO = slot()
            wDO = slot()
            wROv = wRO[:, :].rearrange("p (f n) -> p f n", f=4)
            wDOv = wDO[:, :].rearrange("p (f n) -> p f n", f=4)
            load("pool", wRO, wROv, wro_d, wro_d.h.rearrange("(f p) n -> p f n", p=128))
            load("pool", wDO, wDOv, wdo_d, wdo_d.h.rearrange("(f p) n -> p f n", p=128))
        conv_batch(6)
        pjb = [pm, scb[0], scb[1], scb[2]]
        pji = 0
        for (wx, isq) in ((wq, True), (wk, False)):
            for c in range(4):
                pb = pjb[pji % 4]
                pji += 1
                for k in range(8):
                    S.op("pe", lambda e, wx=wx, k=k, c=c, pb=pb: e.matmul(pb[:], lhsT=wx[:, k, :],
                                                                         rhs=hT[:, k, c * 512:(c + 1) * 512],
                                                                         start=(k == 0), stop=(k == 7)),
                         reads=[w] + [(hT, 4 * c + i) for i in range(4)], writes=[pb], sig=(k == 7))
                if isq:
                    S.op("act", lambda e, c=c, pb=pb: e.activation(out=qT_d[:, c * 512:(c + 1) * 512], in_=pb[:],
                                                                   func=AF.Copy, scale=0.125), reads=[pb],
                         writes=[(qT_d, c)])
                else:
                    for m in range(2):
                        S.op("dve" if m == 0 else "act", lambda e, c=c, m=m, pb=pb: (
                            e.tensor_copy(out=kT_d[m * 64:(m + 1) * 64, m, c * 512:(c + 1) * 512],
                                          in_=pb[m * 64:(m + 1) * 64, :]) if m == 0 else
                            e.activation(out=kT_d[m * 64:(m + 1) * 64, m, c * 512:(c + 1) * 512],
                                         in_=pb[m * 64:(m + 1) * 64, :], func=AF.Copy)), reads=[pb],
                            writes=[(kT_d, (c, m))])
        for g in range(4):
            pb = pjb[pji % 4]
            pji += 1
            for j in range(4):
                t = 4 * g + j
                for k in range(8):
                    S.op("pe", lambda e, k=k, t=t, j=j, pb=pb: e.matmul(pb[:, j * 128:(j + 1) * 128],
                                                                       lhsT=hT[:, k, t * 128:(t + 1) * 128],
                                                                       rhs=wvv[:, k, :], start=(k == 0), stop=(k == 7)),
                         reads=[w, (hT, t)], writes=[pb], sig=(k == 7 and j == 3))
            S.op("act", lambda e, g=g, pb=pb: e.activation(out=v_aug[:, 4 * g:4 * g + 4, 0:128],
                                                           in_=pb[:, :].rearrange("p (a b) -> p a b", a=4), func=AF.Copy),
                 reads=[pb], writes=[(v_aug, g)])
        def qk_op(c, kb, m):
            sc = scb[(2 * kb + m) % 3]
            S.op("pe", lambda e: e.matmul(
                sc[:], lhsT=kT_d[:, m, kb * 128:(kb + 1) * 128],
                rhs=qT_d[:, c * 512:(c + 1) * 512], start=True, stop=True),
                reads=[(kT_d, (kb // 4, m)), (qT_d, c)], writes=[sc])

        def exp_op(c, kb, m):
            sc = scb[(2 * kb + m) % 3]
            pT = pT_sb[m][kb % 2]
            blocks = list(range(4 * c, 4 * c + 4))
            far_r = [g for g in blocks if g < kb - 1]
            near = [g for g in blocks if abs(g - kb) <= 1]
            far_l = [g for g in blocks if g > kb + 1]
            bt = None
            if near:
                c0 = (near[0] - 4 * c) * 128
                c1 = (near[-1] - 4 * c + 1) * 128
                j0 = (near[0] - kb + 1) * 128
                bt = btmp[btc[0] % 2]
                btc[0] += 1
                S.op("dve", lambda e: e.tensor_tensor(
                    out=bt[:, 0:c1 - c0], in0=sc[:, c0:c1], in1=biasT_sb[:, h, j0:j0 + c1 - c0], op=ALU.add),
                    reads=[sc, biasT_sb], writes=[bt])
            for (reg, bi) in ((far_r, h), (far_l, 4 + h)):
                if reg:
                    r0 = (reg[0] - 4 * c) * 128
                    r1 = (reg[-1] - 4 * c + 1) * 128
                    S.op("act", lambda e, r0=r0, r1=r1, bi=bi: e.activation(
                        out=pT[:, r0:r1], in_=sc[:, r0:r1], func=AF.Exp, bias=bfar_sb[:, bi:bi + 1], scale=1.0),
                        reads=[sc, bfar_sb], writes=[pT])
            if near:
                S.op("act", lambda e: e.activation(out=pT[:, c0:c1], in_=bt[:, 0:c1 - c0], func=AF.Exp),
                     reads=[bt], writes=[pT])

        def pv_op(c, kb, m):
            pT = pT_sb[m][kb % 2]
            S.op("pe", lambda e: e.matmul(Ob[m][:], lhsT=v_aug[:, kb, 0:128], rhs=pT[:], start=(kb == 0),
                                          stop=(kb == NT - 1)), reads=[pT, (v_aug, kb // 4)], writes=[Ob[m]])
            S.op("pe", lambda e: e.matmul(Rb[m][:], lhsT=ones_b[:], rhs=pT[:], start=(kb == 0),
                                          stop=(kb == NT - 1)), reads=[pT, ones_b], writes=[Rb[m]])

        def norm_a(c):
            for m in range(2):
                S.op("dve", lambda e, m=m: e.tensor_copy(out=osb[m][:], in_=Ob[m][:]), reads=[Ob[m]], writes=[osb[m]])
                S.op("act", lambda e, m=m: e.activation(out=recb[m][:], in_=Rb[m][:], func=AF.Ln), reads=[Rb[m]],
                     writes=[recb[m]])
            for m in range(2):
                S.op("act", lambda e, m=m: e.activation(out=recb[m][:], in_=recb[m][:], func=AF.Exp, scale=-1.0),
                     reads=[recb[m]], writes=[recb[m]])
                S.op("dve", lambda e, m=m: e.tensor_tensor(out=osb[m][:], in0=osb[m][:], in1=recb[m][:], op=ALU.mult),
                     reads=[osb[m], recb[m]], writes=[osb[m]])
            S.op("dve", lambda e: e.scalar_tensor_tensor(out=ydT[:], in0=osb[1][:], scalar=neglam[:, 0:1], in1=osb[0][:],
                                                         op0=ALU.mult, op1=ALU.add), reads=[osb[0], osb[1], neglam],
                 writes=[ydT])
            S.op("dve", lambda e: e.tensor_tensor(out=ysqT[:], in0=ydT[:], in1=ydT[:], op=ALU.mult), reads=[ydT],
                 writes=[ysqT])

        def norm_b(hh, c):
            S.op("pe", lambda e: e.matmul(prb[:], lhsT=ones_f[:], rhs=ysqT[:], start=True, stop=True),
                 reads=[ones_f, ysqT], writes=[prb])
            S.op("act", lambda e: e.activation(out=rstdT[:], in_=prb[:], func=AF.Ln, scale=1.0 / 128, bias=eps_c[:, 0:1]),
                 reads=[prb, eps_c], writes=[rstdT])
            S.op("act", lambda e: e.activation(out=rstdT[:], in_=rstdT[:], func=AF.Exp, scale=-0.5), reads=[rstdT],
                 writes=[rstdT])
            S.op("dve", lambda e: e.scalar_tensor_tensor(
                out=yT[:, 4 + hh, c * 512:(c + 1) * 512], in0=ydT[:], scalar=sublnT[:, 0:1], in1=rstdT[:], op0=ALU.mult,
                op1=ALU.mult), reads=[ydT, sublnT, rstdT], writes=[(yT, (4 + hh, c))])

        for c in range(4):
            its = [(kb, m) for kb in range(NT) for m in range(2)]
            LOOK = 2
            for i in range(min(LOOK, len(its))):
                qk_op(c, *its[i])
            for i, (kb, m) in enumerate(its):
                exp_op(c, kb, m)
                if i + LOOK < len(its):
                    qk_op(c, *its[i + LOOK])
                pv_op(c, kb, m)
                if i == 8 and pend_norm:
                    norm_b(*pend_norm.pop())
            norm_a(c)
            pend_norm.append((h, c))
    while pend_norm:
        norm_b(*pend_norm.pop())
    debug_dump("yTd", yT, [128, 8, S_LEN], BF16)
    S.pop()
    if stop_after == "D":
        S.pop()
        return finish(nc, S, ins, dbg_out, final_reads)

    S.push()
    mergedT = S.sb("mergedT", [128, 8, S_LEN], BF16)
    conv_batch(3 * NE)
    pG = [[S.ps("pG%d%d" % (a, i), [128, 512], F32) for i in range(2)] for a in range(4)]
    S.push()
    wGs = [S.sb("wG%d" % i, [128, 8, 2, 128], BF16) for i in range(2)]
    sg = [S.sb("sg%d" % i, [128, 512], F32) for i in range(2)]
    m1 = S.sb("m1", [128, 512], F32)
    m2 = S.sb("m2", [128, 512], F32)
    it = 0
    for j in range(8):
        wG = wGs[j % 2]
        load("pool", wG, wG[:, :, 0, :], win_d, win_v[:, :, 3584 + j * 128:3584 + (j + 1) * 128])
        load("pool", wG, wG[:, :, 1, :], win_d, win_v[:, :, 4608 + j * 128:4608 + (j + 1) * 128])
        for c in range(4):
            b = it % 2
            it += 1
            pg, pgd, pR, pDd = pG[0][b], pG[1][b], pG[2][b], pG[3][b]
            csl = slice(c * 512, (c + 1) * 512)
            hreads = [(hT, 4 * c + i) for i in range(4)]
            for gi, pp in ((0, pg), (1, pgd)):
                for k in range(8):
                    S.op("pe", lambda e, pp=pp, gi=gi, k=k, wG=wG, csl=csl: e.matmul(
                        pp[:], lhsT=wG[:, k, gi, :], rhs=hT[:, k, csl], start=(k == 0), stop=(k == 7)),
                        reads=[wG] + hreads, writes=[pp], sig=(k == 7))
            for (pp, wv_, base) in ((pR, wROv, 0), (pDd, wDOv, 4)):
                for f in range(4):
                    S.op("pe", lambda e, pp=pp, wv_=wv_, base=base, f=f, j=j, csl=csl: e.matmul(
                        pp[:], lhsT=wv_[:, f, j * 128:(j + 1) * 128], rhs=yT[:, base + f, csl], start=(f == 0),
                        stop=(f == 3)), reads=[wRO, wDO, yT], writes=[pp], sig=(f == 3))
            S.op("act", lambda e, pg=pg: e.activation(out=sg[0][:], in_=pg[:], func=AF.Sigmoid), reads=[pg], writes=[sg[0]])
            S.op("act", lambda e, pgd=pgd: e.activation(out=sg[1][:], in_=pgd[:], func=AF.Sigmoid), reads=[pgd],
                 writes=[sg[1]])
            S.op("dve", lambda e, pR=pR: e.tensor_tensor(out=m1[:], in0=pR[:], in1=sg[0][:], op=ALU.mult),
                 reads=[pR, sg[0]], writes=[m1])
            S.op("dve", lambda e, pDd=pDd: e.tensor_tensor(out=m2[:], in0=pDd[:], in1=sg[1][:], op=ALU.mult),
                 reads=[pDd, sg[1]], writes=[m2])
            S.op("dve", lambda e, j=j, csl=csl: e.tensor_tensor(out=mergedT[:, j, csl], in0=m1[:], in1=m2[:], op=ALU.add),
                 reads=[m1, m2], writes=[(mergedT, (j, c))])
    debug_dump("mergedT", mergedT, [128, 8, S_LEN], BF16)
    S.pop()
    if stop_after == "G":
        S.pop()
        S.pop()
        return finish(nc, S, ins, dbg_out, final_reads)

    wO = S.sb("wO", [128, 8, D], BF16)
    load("pool", wO, wO[:], wo_d, wo_d.h.rearrange("(k p) n -> p k n", p=128))
    wr = S.sb("wr", [128, 8, NE], BF16)
    load("pool", wr, wr[:], wr_d, wr_d.h.rearrange("(k p) n -> p k n", p=128))
    xt2 = [S.sb("xt2_%d" % i, [128, D], F32) for i in range(2)]
    x1 = [S.sb("x1_%d" % i, [128, D], F32) for i in range(3)]
    nb2 = dict(junk=S.sb("junk2", [128, D], BF16), xn=[S.sb("xn2_%d" % i, [128, D], BF16) for i in range(2)])
    ss2 = S.sb("ss2", [128, NT], F32)
    rstd2 = S.sb("rstd2", [128, NT], F32)
    tmpT2 = S.sb("tmpT2", [128, 8, 128], F32)
    esb = [S.sb("esb%d" % i, [128, NE], F32) for i in range(2)]
    rs = [S.sb("rs%d" % i, [128, 2], F32) for i in range(2)]
    pW = pG[0]
    pT2 = pG[1]
    pL = pG[2][0]
    xns = {}

    def w_s1(t):
        xb = xt2[t % 2]
        x1b = x1[t % 3]
        tsl = slice(t * 128, (t + 1) * 128)
        load("sp", xb, xb[:], x_d, x_d[tsl, :])
        for half in range(2):
            pw = pW[half]
            hs = slice(half * 512, (half + 1) * 512)
            for k in range(8):
                S.op("pe", lambda e, pw=pw, k=k, hs=hs: e.matmul(pw[:], lhsT=mergedT[:, k, tsl], rhs=wO[:, k, hs],
                                                                start=(k == 0), stop=(k == 7)),
                     reads=[mergedT, wO], writes=[pw], sig=(k == 7))
            S.op("dve", lambda e, pw=pw, hs=hs: e.tensor_tensor(out=x1b[:, hs], in0=pw[:], in1=ga_bc[:, 0, hs],
                                                               op=ALU.mult), reads=[pw, ga_bc], writes=[x1b])
            S.op("pool", lambda e, hs=hs: e.tensor_tensor(out=x1b[:, hs], in0=x1b[:, hs], in1=xb[:, hs], op=ALU.add),
                 reads=[x1b, xb], writes=[x1b])
        S.op("sp", lambda e: e.dma_start(out=acc_d[tsl, :], in_=x1b[:]), reads=[x1b], writes=[(acc_d, t)], dma=True)

    def w_s2(t):
        x1b = x1[t % 3]
        tsl = slice(t * 128, (t + 1) * 128)
        xn = norm_stats(x1b, t, ss2, rstd2, nb2)
        S.op("sp", lambda e: e.dma_start(out=xn2_d[tsl, :], in_=xn[:]), reads=[xn], writes=[(xn2_d, t)], dma=True)
        xns[t] = xn

    def w_s3(t):
        tsl = slice(t * 128, (t + 1) * 128)
        transp_evac(xns[t], t, 1, hT, pT2, tmpT2, act_evac=False)
        for k in range(8):
            S.op("pe", lambda e, k=k: e.matmul(pL[:, 0:NE], lhsT=hT[:, k, tsl], rhs=wr[:, k, :], start=(k == 0),
                                               stop=(k == 7)), reads=[(hT, t), wr], writes=[pL], sig=(k == 7))
        es_ = esb[t % 2]
        rs_ = rs[t % 2]
        S.op("act", lambda e: e.activation(out=es_[:], in_=pL[:, 0:NE], func=AF.Exp, accum_out=rs_[:, 0:1]),
             reads=[pL], writes=[es_, rs_])
        S.op("dve", lambda e: e.reciprocal(out=rs_[:, 1:2], in_=rs_[:, 0:1]), reads=[rs_], writes=[rs_])
        S.op("dve", lambda e: e.tensor_scalar(out=aff[:, t, :], in0=es_[:], scalar1=rs_[:, 1:2], scalar2=None,
                                              op0=ALU.mult), reads=[es_, rs_], writes=[(aff, t)])

    w_s1(0)
    w_s1(1)
    w_s2(0)
    for t in range(NT):
        if t + 2 < NT:
            w_s1(t + 2)
        if t + 1 < NT:
            w_s2(t + 1)
        w_s3(t)
    debug_dump("aff", aff, [128, NT, NE])
    debug_dump("h2T", hT, [128, 8, S_LEN], BF16)
    S.pop()
    if stop_after == "W":
        S.pop()
        return finish(nc, S, ins, dbg_out, final_reads)

    S.push()
    affT = S.sb("affT", [NE, S_LEN], F32)
    junkK = S.sb("junkK", [NE, S_LEN], F32)
    maskf = S.sb("maskf", [NE, S_LEN], F32)
    posf = S.sb("posf", [NE, S_LEN], F32)
    thr = S.sb("thr", [NE, 4], F32)
    pK = [S.ps("pK%d" % i, [128, 512], F32) for i in range(4)]
    for g in range(4):
        pk = pK[g]
        for j in range(4):
            t = 4 * g + j
            S.op("pe", lambda e, pk=pk, j=j, t=t: e.transpose(out=pk[0:NE, j * 128:(j + 1) * 128], in_=aff[:, t, :],
                                                              identity=ident_f[:]), reads=[(aff, t), ident_f],
                 writes=[pk], sig=(j == 3))
        S.op("act", lambda e, pk=pk, g=g: e.activation(out=affT[:, g * 512:(g + 1) * 512], in_=pk[0:NE, :], func=AF.Copy),
             reads=[pk], writes=[affT])
    NIT = 26
    S.op("dve", lambda e: e.memset(thr[:, 1:2], 0.5), writes=[thr])
    for itn in range(NIT):
        step = 0.5 ** (itn + 1)
        S.op("dve", lambda e: e.tensor_scalar(out=junkK[:], in0=affT[:], scalar1=thr[:, 1:2], scalar2=0.0, op0=ALU.is_ge,
                                              op1=ALU.add, accum_out=thr[:, 2:3]), reads=[affT, thr], writes=[junkK, thr])
        if itn < NIT - 1:
            S.op("dve", lambda e, step=step: e.tensor_scalar(out=thr[:, 3:4], in0=thr[:, 2:3], scalar1=float(CAP),
                                                            scalar2=step, op0=ALU.is_ge, op1=ALU.mult), reads=[thr],
                 writes=[thr])
            S.op("dve", lambda e, step=step: e.scalar_tensor_tensor(out=thr[:, 1:2], in0=thr[:, 1:2], scalar=-0.5 * step,
                                                                   in1=thr[:, 3:4], op0=ALU.add, op1=ALU.add),
                 reads=[thr], writes=[thr])
        else:
            S.op("dve", lambda e, step=step: e.tensor_scalar(out=thr[:, 3:4], in0=thr[:, 2:3], scalar1=float(CAP),
                                                            scalar2=step, op0=ALU.is_lt, op1=ALU.mult), reads=[thr],
                 writes=[thr])
            S.op("dve", lambda e: e.tensor_tensor(out=thr[:, 0:1], in0=thr[:, 1:2], in1=thr[:, 3:4], op=ALU.subtract),
                 reads=[thr], writes=[thr])
    S.op("dve", lambda e: e.tensor_scalar(out=maskf[:], in0=affT[:], scalar1=thr[:, 0:1], scalar2=None, op0=ALU.is_ge),
         reads=[affT, thr], writes=[maskf])
    S.op("dve", lambda e: e.memset(junkK[:], 1.0), writes=[junkK])
    S.op("dve", lambda e: e.tensor_tensor_scan(out=posf[:], data0=junkK[:], data1=maskf[:], initial=0.0, op0=ALU.mult,
                                               op1=ALU.add), reads=[junkK, maskf], writes=[posf])
    S.op("dve", lambda e: e.tensor_tensor(out=posf[:], in0=posf[:], in1=maskf[:], op=ALU.mult), reads=[posf, maskf],
         writes=[posf])
    S.op("dve", lambda e: e.tensor_scalar(out=posf[:], in0=posf[:], scalar1=-1.0, scalar2=None, op0=ALU.add), reads=[posf],
         writes=[posf])
    debug_dump("posf", posf, [NE, S_LEN])
    pk = pK[0]
    for t in range(NT):
        S.op("pe", lambda e, t=t: e.transpose(out=pk[:, t * NE:(t + 1) * NE], in_=posf[:, t * 128:(t + 1) * 128],
                                              identity=ident_f[0:NE, 0:NE]), reads=[posf, ident_f], writes=[pk],
             sig=(t == NT - 1))
    S.op("act", lambda e: e.activation(out=posTM[:].rearrange("p a b -> p (a b)"), in_=pk[:, 0:NT * NE], func=AF.Copy),
         reads=[pk], writes=[posTM])
    S.pop()
    S.pop()

    S.push()
    iota_f = S.sb("iota_f", [128, 256], F32)
    pt_sb = S.sb("pt_sb", [128, NT, 2], F32)
    load("sp", iota_f, iota_f[:], iota_d, iota_d.h)
    load("sp", pt_sb, pt_sb[:], pt_d, pt_d.h)
    S.op("dve", lambda e: e.tensor_copy(out=iota_b[:], in_=iota_f[:]), reads=[iota_f], writes=[iota_b])
    Rr = S.sb("Rr", [128, NT, NE, 4], BF16)
    tmpf = S.sb("tmpf", [128, NT, NE], F32)
    S.op("dve", lambda e: e.tensor_copy(out=Rr[:, :, :, 0:2], in_=pt_sb[:].unsqueeze(2).to_broadcast([128, NT, NE, 2])),
         reads=[pt_sb], writes=[Rr])
    S.op("dve", lambda e: e.tensor_copy(out=Rr[:, :, :, 2], in_=aff[:]), reads=[aff], writes=[Rr])
    S.op("dve", lambda e: e.tensor_tensor(out=tmpf[:], in0=aff[:], in1=Rr[:, :, :, 2], op=ALU.subtract), reads=[aff, Rr],
         writes=[tmpf])
    S.op("dve", lambda e: e.tensor_copy(out=Rr[:, :, :, 3], in_=tmpf[:]), reads=[tmpf], writes=[Rr])
    oh = [S.sb("oh%d" % i, [128, NT, 256], BF16) for i in range(2)]
    pix = S.sb("pix", [128, 2, 4], F32)
    idxf = S.sb("idxf", [128, 2 * NE], F32)
    wgt = [[S.sb("wE%d_%d" % (a, i), [128, 8, D], BF16) for i in range(2)] for a in range(3)]
    xin = [[S.sb("xin%d_%d" % (i, j), [128, D], BF16) for j in range(2)] for i in range(2)]
    xinT = [S.sb("xinT%d" % i, [128, 8, 256], BF16) for i in range(2)]
    hidT = S.sb("hidT", [128, 8, 256], BF16)
    sa = [S.sb("sa%d" % i, [128, 256], F32) for i in range(2)]
    ysb = [[S.sb("ysb%d_%d" % (i, j), [128, D], F32) for j in range(2)] for i in range(2)]
    tmpT3 = S.sb("tmpT3", [128, 8, 128], F32)
    pI0 = S.ps("pI0", [128, 512], F32)
    pI = [pI0, pI0]
    pYb = [S.ps("pY%d" % i, [128, 512], F32) for i in range(2)]
    pX = S.ps("pX", [128, 512], F32)
    pAU = [[S.ps("pAU%d%d" % (a, i), [128, 512], F32) for i in range(2)] for a in range(2)]
    pY = pYb
    wsrc = (wg_d, wu_d, wd_d)

    bcreg = nc.gpsimd.alloc_register("bcreg")
    nc.gpsimd.reg_mov(bcreg, S_LEN - 1)

    def load_w(e_):
        for a in range(3):
            wt = wgt[a][e_ % 2]
            S.op("sp", lambda e, wt=wt, a=a, e_=e_: e.dma_start(
                out=wt[:].rearrange("p k n -> p (k n)"), in_=wb_d[a].h[e_]), reads=[(wb_d[a], e_)], writes=[wt],
                dma=True)

    def gather(e_):
        for s2 in range(2):
            col = e_ * 2 + s2
            S.op("pool", lambda e, e_=e_, s2=s2, col=col: e.indirect_dma_start(
                out=xin[e_ % 2][s2][:], out_offset=None, in_=xn2_d.h,
                in_offset=bass.IndirectOffsetOnAxis(ap=idx_c[col][:, 0:1], axis=0), bounds_check=bcreg,
                oob_is_err=False), reads=[idx_c[col], xn2_d], writes=[xin[e_ % 2][s2]], dma=True)

    def scatter(e_):
        for s2 in range(2):
            col = e_ * 2 + s2
            S.op("pool", lambda e, e_=e_, s2=s2, col=col: e.indirect_dma_start(
                out=acc_d.h, out_offset=bass.IndirectOffsetOnAxis(ap=idx_c[col][:, 0:1], axis=0),
                in_=ysb[e_ % 2][s2][:], in_offset=None, bounds_check=bcreg, oob_is_err=True,
                compute_op=ALU.add), reads=[idx_c[col], ysb[e_ % 2][s2]], writes=[acc_d], dma=True)

    def bi_onehot(e_):
        ohe = oh[e_ % 2]
        S.op("dve", lambda e: e.tensor_tensor(out=ohe[:], in0=bcm(iota_b[:], NT), in1=bc3(posTM[:, :, e_], 256),
                                              op=ALU.is_equal), reads=[iota_b, posTM], writes=[ohe])

    def bi_mm(e_):
        ohe = oh[e_ % 2]
        for s2 in range(2):
            pi = pI[s2]
            for t in range(NT):
                S.op("pe", lambda e, pi=pi, t=t, s2=s2: e.matmul(
                    pi[:, 0:4], lhsT=ohe[:, t, s2 * 128:(s2 + 1) * 128], rhs=Rr[:, t, e_, :], start=(t == 0),
                    stop=(t == NT - 1)), reads=[ohe, Rr], writes=[pi], sig=(t == NT - 1))
            S.op("act", lambda e, pi=pi, s2=s2: e.activation(out=pix[:, s2, :], in_=pi[:, 0:4], func=AF.Copy), reads=[pi],
                 writes=[(pix, s2)])
            col = e_ * 2 + s2
            S.op("dve", lambda e, s2=s2, col=col: e.scalar_tensor_tensor(
                out=idxf[:, col:col + 1], in0=pix[:, s2, 1:2], scalar=128.0, in1=pix[:, s2, 0:1], op0=ALU.mult,
                op1=ALU.add), reads=[(pix, s2)], writes=[(idxf, col)])
            S.op("dve", lambda e, s2=s2, col=col: e.tensor_tensor(out=gate[:, col:col + 1], in0=pix[:, s2, 2:3],
                                                                 in1=pix[:, s2, 3:4], op=ALU.add), reads=[(pix, s2)],
                 writes=[(gate, col)])
            S.op("dve", lambda e, col=col: e.tensor_copy(out=idx_c[col][:], in_=idxf[:, col:col + 1]),
                 reads=[(idxf, col)], writes=[idx_c[col]])

    def build_index(e_):
        bi_onehot(e_)
        bi_mm(e_)

    tmpT3b = [tmpT3, tmpT3]
    pXb = [pX, pI0]

    def prep_xT(e_):
        b = e_ % 2
        xT = xinT[b]
        for s2 in range(2):
            pxk = pXb[s2]
            pxv = pview(pxk, BF16, 8, 128)
            tt = tmpT3b[s2]
            for j in range(8):
                S.op("pe", lambda e, pxv=pxv, j=j, s2=s2: e.transpose(out=pxv[:, j, :],
                                                                    in_=xin[b][s2][:, j * 128:(j + 1) * 128],
                                                                    identity=ident_b[:]),
                     reads=[xin[b][s2], ident_b], writes=[pxk], sig=(j == 7))
            S.op("dve", lambda e, pxv=pxv, tt=tt: e.tensor_tensor(out=tt[:], in0=pxv, in1=bc3(gscT[:, 1, :], 128),
                                                                 op=ALU.mult), reads=[pxk, gscT], writes=[tt])
            S.op("dve", lambda e, s2=s2, tt=tt: e.tensor_tensor(out=xT[:, :, s2 * 128:(s2 + 1) * 128], in0=tt[:],
                                                               in1=bc3(modT[:, 2, :], 128), op=ALU.add),
                 reads=[tt, modT], writes=[(xT, s2)])

    load_w(0)
    build_index(0)
    gather(0)
    load_w(1)
    build_index(1)
    gather(1)
    prep_xT(0)
    for e_ in range(NE):
        b = e_ % 2
        wg_, wu_, wd_ = wgt[0][b], wgt[1][b], wgt[2][b]
        xT = xinT[b]
        if e_ + 2 < NE:
            bi_onehot(e_ + 2)
        for fc in range(8):
            pa = pAU[0][fc % 2]
            pu = pAU[1][fc % 2]
            fs = slice(fc * 128, (fc + 1) * 128)
            for (pp, wt) in ((pa, wg_), (pu, wu_)):
                for k in range(8):
                    S.op("pe", lambda e, pp=pp, wt=wt, k=k, fs=fs, xT=xT: e.matmul(pp[:, 0:256], lhsT=wt[:, k, fs],
                                                                                rhs=xT[:, k, :], start=(k == 0),
                                                                                stop=(k == 7)),
                         reads=[wt, xT], writes=[pp], sig=(k == 7))
            sab = sa[fc % 2]
            S.op("act", lambda e, pa=pa, sab=sab: e.activation(out=sab[:], in_=pa[:, 0:256], func=AF.Silu), reads=[pa],
                 writes=[sab])
            S.op("dve", lambda e, pu=pu, sab=sab, fc=fc: e.tensor_tensor(out=hidT[:, fc, :], in0=pu[:, 0:256], in1=sab[:],
                                                                        op=ALU.mult), reads=[pu, sab],
                 writes=[(hidT, fc)])
        if e_ + 2 < NE:
            bi_mm(e_ + 2)
        if e_ + 1 < NE:
            prep_xT(e_ + 1)
        yb = ysb[b]
        for s2 in range(2):
            for dh in range(2):
                py = pY[(s2 * 2 + dh) % 2]
                ds = slice(dh * 512, (dh + 1) * 512)
                for fc in range(8):
                    S.op("pe", lambda e, py=py, fc=fc, s2=s2, ds=ds, wd_=wd_: e.matmul(
                        py[:], lhsT=hidT[:, fc, s2 * 128:(s2 + 1) * 128], rhs=wd_[:, fc, ds], start=(fc == 0),
                        stop=(fc == 7)), reads=[hidT, wd_], writes=[py], sig=(fc == 7))
                col = e_ * 2 + s2
                S.op("dve", lambda e, py=py, yb=yb, s2=s2, ds=ds, col=col: e.scalar_tensor_tensor(
                    out=yb[s2][:, ds], in0=py[:], scalar=gate[:, col:col + 1], in1=ga_bc[:, 1, ds], op0=ALU.mult,
                    op1=ALU.mult), reads=[py, (gate, col), ga_bc], writes=[yb[s2]])
        scatter(e_)
        if e_ + 2 < NE:
            load_w(e_ + 2)
            gather(e_ + 2)
    debug_dump("idxf", idxf, [128, 2 * NE])
    debug_dump("gate", gate, [128, 2 * NE])
    S.pop()

    S.push()
    xf = [S.sb("xf%d" % i, [128, D], F32) for i in range(4)]
    of_ = [S.sb("of%d" % i, [128, D], F32) for i in range(3)]
    junkF = S.sb("junkF", [128, D], BF16)
    ssF = S.sb("ssF", [128, NT], F32)
    rsF = S.sb("rsF", [128, NT], F32)
    def fload(t):
        xb = xf[t % 4]
        tsl = slice(t * 128, (t + 1) * 128)
        S.op("sp", lambda e: e.dma_start(out=xb[:], in_=acc_d[tsl, :]), reads=[acc_d], writes=[xb], dma=True)

    for t in range(4):
        fload(t)
    for t in range(NT):
        xb = xf[t % 4]
        ob = of_[t % 3]
        tsl = slice(t * 128, (t + 1) * 128)
        S.op("act", lambda e, xb=xb, t=t: e.activation(out=junkF[:], in_=xb[:], func=AF.Square, accum_out=ssF[:, t:t + 1]),
             reads=[xb], writes=[junkF, (ssF, t)])
        S.op("act", lambda e, t=t: e.activation(out=ssF[:, t:t + 1], in_=ssF[:, t:t + 1], func=AF.Ln, scale=1.0 / D,
                                                bias=eps_g[:, 0:1]), reads=[(ssF, t), eps_g], writes=[(ssF, t)])
        S.op("act", lambda e, t=t: e.activation(out=rsF[:, t:t + 1], in_=ssF[:, t:t + 1], func=AF.Exp, scale=-0.5),
             reads=[(ssF, t)], writes=[(rsF, t)])
        S.op("dve", lambda e, xb=xb, ob=ob, t=t: e.scalar_tensor_tensor(out=ob[:], in0=xb[:], scalar=rsF[:, t:t + 1],
                                                                       in1=fing_bc[:], op0=ALU.mult, op1=ALU.mult),
             reads=[xb, (rsF, t), fing_bc], writes=[ob])
        S.op("act", lambda e, ob=ob, tsl=tsl: e.dma_start(out=out_d[tsl, :], in_=ob[:]), reads=[ob], writes=[(out_d, t)],
             dma=True)
        if t + 4 < NT:
            fload(t + 4)
    S.pop()

    return finish(nc, S, ins, dbg_out, final_reads)


def finish(nc, S, ins, dbg_out, final_reads):
    S.op("sp", lambda e: e.nop(), reads=final_reads)
    S.emit()
    S.stack.close()
    return nc, dbg_out


_TABS = None


def _prep_inputs(inputs, tabs):
    f32 = np.float32
    g = lambda k: np.ascontiguousarray(np.asarray(inputs[k], dtype=f32))
    x = g("x")
    c = g("c")
    B = x.shape[0]
    rel_bias = g("rel_bias")
    biasT = np.ascontiguousarray(rel_bias[tabs["bidx"]].transpose(0, 3, 1, 2).reshape(128, 4, 384))
    bfar = np.zeros((128, 8), f32)
    bfar[:, 0:4] = rel_bias[31][None, :]
    bfar[:, 4:8] = rel_bias[15][None, :]
    lam4 = np.concatenate([g("lambda_q1")[0], g("lambda_k1")[0], g("lambda_q2")[0], g("lambda_k2")[0]])[None, :]
    shared = {
        "w_ada": g("w_ada")[0], "b_ada": g("b_ada"),
        "g1T": np.ascontiguousarray(g("norm_mix_g")[0].reshape(8, 128).T),
        "g2T": np.ascontiguousarray(g("norm_ffn_g")[0].reshape(8, 128).T),
        "final_g": g("final_g")[None, :], "w_in": g("w_in")[0],
        "ret_gn_g": g("ret_gn_g"), "subln_g": g("diff_subln_g"), "lam4": np.ascontiguousarray(lam4),
        "w_ret_out": g("w_ret_out")[0], "w_diff_out": g("w_diff_out")[0], "w_o": g("w_o")[0],
        "biasT": biasT, "bfar": bfar, "w_router": g("w_router")[0],
        "w_gate": g("w_exp_gate")[0], "w_up": g("w_exp_up")[0], "w_down": g("w_exp_down")[0],
        "cosT": tabs["cosT"], "sinT": tabs["sinT"], "maskT": tabs["maskT"], "kdec": tabs["kdec"],
        "qdec": tabs["qdec"], "ident": tabs["ident"], "iota": tabs["iota"], "pt": tabs["pt"],
    }
    in_maps = []
    for b in range(B):
        m = dict(shared)
        m["x"] = np.ascontiguousarray(x[b])
        m["cT"] = np.ascontiguousarray(c[b].reshape(8, 128).T)
        in_maps.append(m)
    return in_maps


def run(inputs, stop_after=None, dbg=None, trace=False, cores=8):
    global _TABS
    if _TABS is None:
        _TABS = _host_tables()
    nc, dbg_out = build_program(_TABS, stop_after=stop_after, dbg=dbg)
    in_maps = _prep_inputs(inputs, _TABS)[:cores]
    used = set(nc_input_names(nc))
    in_maps = [{k: v for k, v in m.items() if k in used} for m in in_maps]
    res = run_bass_kernel_spmd(nc, in_maps, core_ids=list(range(len(in_maps))), trace=trace)
    return res


def nc_input_names(nc):
    return _INPUT_NAMES


_INPUT_NAMES = ["x", "cT", "w_ada", "b_ada", "g1T", "g2T", "final_g", "w_in", "ret_gn_g", "subln_g", "lam4",
                "w_ret_out", "w_diff_out", "w_o", "biasT", "bfar", "w_router", "w_gate", "w_up", "w_down",
                "cosT", "sinT", "maskT", "kdec", "qdec", "ident", "iota", "pt"]


def kernel(**inputs):
    res = run(inputs)
    out = np.stack([np.asarray(r["out"], dtype=np.float32) for r in res.results], axis=0)
    return out
```

```python
import math
import numpy as np
import ml_dtypes
from contextlib import ExitStack
import concourse.bass as bass
import concourse.mybir as mybir
from concourse.bass_utils import run_bass_kernel_spmd

F32 = mybir.dt.float32
BF16 = mybir.dt.bfloat16
I32 = mybir.dt.int32
AF = mybir.ActivationFunctionType
ALU = mybir.AluOpType
AX = mybir.AxisListType

D = 1024
S_LEN = 2048
NT = 16
NE = 16
CAP = 256
EPS = 1e-6


class _St:
    __slots__ = ("w", "r")

    def __init__(self):
        self.w = None
        self.r = {}


class Tile:
    def __init__(self, h, name, psum=False):
        self.h = h
        self.name = name
        self.psum = psum
        self.whole = _St()
        self.parts = {}

    def __getitem__(self, idx):
        return self.h[idx]

    def states(self, key):
        if key is None:
            return [self.whole] + list(self.parts.values())
        if key not in self.parts:
            self.parts[key] = _St()
        return [self.whole, self.parts[key]]

    def target(self, key):
        if key is None:
            return self.whole
        if key not in self.parts:
            self.parts[key] = _St()
        return self.parts[key]


class Op:
    __slots__ = ("id", "eng", "dma", "sig", "semval", "sem")


import os
_SKIP = set(os.environ.get('KSKIP', '').split(','))


class Sched:
    ENGS = ("pe", "act", "dve", "pool", "sp")

    def __init__(self, nc, ndma_sems=12):
        self.nc = nc
        self.ops = []
        self.stack = ExitStack()
        self.ndma = ndma_sems
        self.freed = []
        self.scopes = []
        st = self.stack
        self.esem = {e: st.enter_context(nc.semaphore("s_" + e)) for e in self.ENGS}
        self.dsem = {e: [st.enter_context(nc.semaphore("d_%s_%d" % (e, i))) for i in range(ndma_sems)]
                     for e in ("sp", "pool", "act")}
        self.cnt = {e: 0 for e in self.ENGS}
        self.dcnt = {e: 0 for e in ("sp", "pool", "act")}
        self.known = {e: {} for e in self.ENGS}
        self.engobj = {"pe": nc.tensor, "act": nc.scalar, "dve": nc.vector, "pool": nc.gpsimd, "sp": nc.sync}
        self.n_waits = 0
        self.pending_pe = []

    def sb(self, name, shape, dtype):
        h = self._ctx().enter_context(self.nc.sbuf_tensor(name, list(shape), dtype))
        return self._mk(h, name)

    def ps(self, name, shape, dtype):
        assert list(shape) == [128, 512] and dtype == F32
        h = self._ctx().enter_context(self.nc.psum_tensor(name, list(shape), dtype))
        return self._mk(h, name, True)

    def _mk(self, h, name, psum=False):
        t = Tile(h, name, psum)
        for (e, oid) in self.freed:
            if e not in t.whole.r or t.whole.r[e] < oid:
                t.whole.r[e] = oid
        if self.scopes:
            self.scopes[-1][1].append(t)
        return t

    def _ctx(self):
        return self.scopes[-1][0] if self.scopes else self.stack

    def push(self):
        self.scopes.append((ExitStack(), []))

    def pop(self):
        es, tiles = self.scopes.pop()
        latest = {}
        keep = []
        for (e, oid) in self.freed:
            if e.startswith("dma"):
                keep.append((e, oid))
            else:
                latest[e] = max(latest.get(e, -1), oid)
        for t in tiles:
            for st in [t.whole] + list(t.parts.values()):
                ids = ([st.w] if st.w is not None else []) + list(st.r.values())
                for oid in ids:
                    o = self.ops[oid]
                    if o.dma:
                        keep.append(("dma%d" % oid, oid))
                    else:
                        latest[o.eng] = max(latest.get(o.eng, -1), oid)
        keep = list({k: v for k, v in keep}.items())
        if len(keep) > 48:
            keep = keep[-48:]
        self.freed = list(latest.items()) + keep
        es.close()

    def op(self, eng, fn, reads=(), writes=(), dma=False, sig=True):
        o = Op()
        o.id = len(self.ops)
        o.eng = eng
        o.dma = dma
        o.sig = sig or dma
        o.sem = None
        o.semval = None
        deps = set()
        key_e = ("dma%d" % o.id) if dma else eng

        def norm(x):
            return x if isinstance(x, tuple) else (x, None)

        if eng != "pe":
            extra = [norm(x)[0] for x in reads if norm(x)[0].psum]
            writes = list(writes) + extra
        for x in reads:
            t, k = norm(x)
            for st in t.states(k):
                if st.w is not None:
                    deps.add(st.w)
        for x in writes:
            t, k = norm(x)
            for st in t.states(k):
                if st.w is not None:
                    deps.add(st.w)
                for e, oid in st.r.items():
                    deps.add(oid)
        for x in reads:
            t, k = norm(x)
            t.target(k).r[key_e] = o.id
        for x in writes:
            t, k = norm(x)
            if k is None:
                for st in t.states(None):
                    st.w = o.id
                    st.r = {}
            else:
                st = t.target(k)
                st.w = o.id
                st.r = {}
        waits = {}
        for d in deps:
            po = self.ops[d]
            if (not po.dma) and (not dma) and po.eng == eng and eng == "pe":
                continue
            assert po.sem is not None, "dependency on non-signalling op (%s)" % po.eng
            key = id(po.sem)
            if key not in waits or waits[key][1] < po.semval:
                waits[key] = (po.sem, po.semval)
        E = self.engobj[eng]
        known = self.known[eng]
        if dma:
            n = self.dcnt[eng]
            self.dcnt[eng] += 1
            o.sem = self.dsem[eng][n % self.ndma]
            o.semval = 16 * (n // self.ndma + 1)
            if n >= self.ndma:
                key = id(o.sem)
                v = 16 * (n // self.ndma)
                if key not in waits or waits[key][1] < v:
                    waits[key] = (o.sem, v)
        for key, (s, v) in waits.items():
            if known.get(key, 0) >= v:
                continue
            E.wait_ge(s, v)
            self.n_waits += 1
            known[key] = v
        ins = fn(E)
        if dma:
            ins.then_inc(o.sem, 16)
        elif o.sig:
            self.cnt[eng] += 1
            o.sem = self.esem[eng]
            o.semval = self.cnt[eng]
            ins.then_inc(o.sem, 1)
            if eng == "pe":
                for q in self.pending_pe:
                    q.sem, q.semval = o.sem, o.semval
                self.pending_pe = []
        elif eng == "pe":
            self.pending_pe.append(o)
        self.ops.append(o)
        return o

    def emit(self):
        pass


def _t5_bucket_np(rel):
    nb = 16
    max_exact = 8
    ret = (rel > 0).astype(np.int32) * nb
    n = np.abs(rel)
    large = max_exact + (np.log(np.maximum(n, 1).astype(np.float32) / max_exact)
                         / math.log(128 / max_exact) * (nb - max_exact)).astype(np.int32)
    large = np.minimum(large, nb - 1)
    return ret + np.where(n < max_exact, n, large)


def _host_tables():
    f32 = np.float32
    heads = np.arange(4, dtype=f32)
    lgf = np.log1p(-np.exp2(-5.0 - heads)).astype(f32)
    lgb = np.log1p(-np.exp2(-5.5 - heads)).astype(f32)
    C = 128
    idx = np.arange(C, dtype=f32)
    inv = (1.0 / (10000.0 ** np.linspace(0.0, 1.0, 64, dtype=f32))).astype(f32)
    pos = np.arange(S_LEN, dtype=f32)
    ang = pos[:, None] * inv[None, :]
    cos = np.cos(ang).astype(f32).reshape(NT, 128, 64).transpose(1, 0, 2)
    sin = np.sin(ang).astype(f32).reshape(NT, 128, 64).transpose(1, 0, 2)
    ksc = f32(128 ** -0.5)
    cosT = np.ascontiguousarray(cos)
    sinT = np.ascontiguousarray(sin)
    diff = idx[:, None] - idx[None, :]
    maskT = np.zeros((4, C, C), f32)
    for h in range(4):
        Mf = np.where(diff >= 0, np.exp(np.maximum(diff, 0.0) * lgf[h]), 0.0)
        Mb = np.where(diff < 0, np.exp(np.maximum(-diff, 0.0) * lgb[h]), 0.0)
        maskT[h] = ((Mf + Mb).astype(f32) * ksc).T
    maskT = np.ascontiguousarray(maskT.transpose(1, 0, 2))
    kdec = np.zeros((C, 8), f32)
    qdec = np.zeros((8, C), f32)
    for h in range(4):
        kdec[:, h] = np.exp((C - 1 - idx) * lgf[h]) * ksc
        kdec[:, 4 + h] = np.exp(idx * lgb[h]) * ksc
        qdec[h] = np.exp((idx + 1) * lgf[h])
        qdec[4 + h] = np.exp((C - idx) * lgb[h])
    qdec_bc = np.ascontiguousarray(np.broadcast_to(qdec.reshape(1, 8 * C), (128, 8 * C))).astype(f32)
    cdec = [float(np.exp(f32(C) * lgf[h])) for h in range(4)] + [float(np.exp(f32(C) * lgb[h])) for h in range(4)]
    ident = np.eye(128, dtype=f32)
    kl = np.arange(128)[:, None, None]
    j = np.arange(3)[None, :, None]
    ql = np.arange(128)[None, None, :]
    bidx = _t5_bucket_np(kl - ql - (j - 1) * 128)
    iota = np.ascontiguousarray(np.broadcast_to(np.arange(256, dtype=f32)[None, :], (128, 256)))
    pt = np.zeros((128, NT, 2), f32)
    pt[:, :, 0] = np.arange(128)[:, None]
    pt[:, :, 1] = np.arange(NT)[None, :]
    return dict(cosT=cosT, sinT=sinT, maskT=maskT, kdec=kdec, qdec=qdec_bc, cdec=cdec, ident=ident,
                bidx=bidx, iota=iota, pt=pt)


def build_program(tabs, stop_after=None, dbg=None):
    nc = bass.Bass("TRN2", target_bir_lowering=False)
    S = Sched(nc)
    dbg = dbg or []
    dbg_out = {}
    ins = {}

    def din(name, shape, dt=F32):
        t = Tile(nc.dram_tensor(name, list(shape), dt, kind="ExternalInput").ap(), name)
        ins[name] = t
        return t

    x_d = din("x", [S_LEN, D])
    cT_d = din("cT", [128, 8])
    wada_d = din("w_ada", [D, 6 * D])
    bada_d = din("b_ada", [1, 6 * D])
    g1T_d = din("g1T", [128, 8])
    g2T_d = din("g2T", [128, 8])
    fing_d = din("final_g", [1, D])
    win_d = din("w_in", [D, 5632])
    gn_d = din("ret_gn_g", [1, 512])
    subln_d = din("subln_g", [1, 128])
    lam_d = din("lam4", [1, 256])
    wro_d = din("w_ret_out", [512, D])
    wdo_d = din("w_diff_out", [512, D])
    wo_d = din("w_o", [D, D])
    biasT_d = din("biasT", [128, 4, 384])
    bfar_d = din("bfar", [128, 8])
    wr_d = din("w_router", [D, NE])
    wg_d = din("w_gate", [NE, D, D])
    wu_d = din("w_up", [NE, D, D])
    wd_d = din("w_down", [NE, D, D])
    cos_d = din("cosT", [128, NT, 64])
    sin_d = din("sinT", [128, NT, 64])
    maskT_d = din("maskT", [128, 4, 128])
    kdec_d = din("kdec", [128, 8])
    qdec_d = din("qdec", [128, 8 * 128])
    ident_d = din("ident", [128, 128])
    iota_d = din("iota", [128, 256])
    pt_d = din("pt", [128, NT, 2])
    out_d = Tile(nc.dram_tensor("out", [S_LEN, D], F32, kind="ExternalOutput").ap(), "out")
    acc_d = Tile(nc.dram_tensor("acc_scr", [S_LEN, D], F32, kind="Internal").ap(), "acc")
    xn2_d = Tile(nc.dram_tensor("xn2_scr", [S_LEN, D], BF16, kind="Internal").ap(), "xn2")
    cdec = tabs["cdec"]
    wb_d = [Tile(nc.dram_tensor("wb_scr%d" % a, [NE, 128, 8 * D], BF16, kind="Internal").ap(), "wb%d" % a) for a in range(3)]
    wsrc_f = (wg_d, wu_d, wd_d)
    conv_i = [0]

    def conv_batch(n):
        for _ in range(n):
            i = conv_i[0]
            if i >= 3 * NE:
                return
            conv_i[0] += 1
            e_, a = i // 3, i % 3
            S.op("pool", lambda e, e_=e_, a=a: e.dma_start(
                out=wb_d[a].h[e_].rearrange("p (k n) -> p k n", k=8),
                in_=wsrc_f[a].h[e_].rearrange("(k p) n -> p k n", p=128)), reads=[wsrc_f[a]], writes=[(wb_d[a], e_)],
                dma=True)
    final_reads = [out_d]

    def debug_dump(name, tile, shape, dt=F32, src=None):
        if name not in dbg:
            return
        t = Tile(nc.dram_tensor("dbg_" + name, list(shape), dt, kind="ExternalOutput").ap(), name)
        dbg_out[name] = t
        S.op("sp", lambda e: e.dma_start(out=t.h, in_=(src if src is not None else tile[:])), reads=[tile], writes=[t], dma=True)
        final_reads.append(t)

    def load(eng, dst, dst_ap, src, src_ap, wkey=None):
        S.op(eng, lambda e: e.dma_start(out=dst_ap, in_=src_ap), reads=[src],
             writes=[(dst, wkey) if wkey is not None else dst], dma=True)

    def pview(bank, dt, a, b):
        ap = bank[:, :]
        if dt == BF16:
            ap = ap.bitcast(BF16)
        return ap[:, 0:a * b].rearrange("p (a b) -> p a b", a=a)

    def bc3(ap2, n):
        return ap2.unsqueeze(2).to_broadcast([ap2.shape[0], ap2.shape[1], n])

    def bcm(ap2, n):
        return ap2.unsqueeze(1).to_broadcast([ap2.shape[0], n, ap2.shape[1]])

    ident_f = S.sb("ident_f", [128, 128], F32)
    ident_b = S.sb("ident_b", [128, 128], BF16)
    ones_b = S.sb("ones_b", [128, 128], BF16)
    load("sp", ident_f, ident_f[:], ident_d, ident_d.h)
    S.op("dve", lambda e: e.tensor_copy(out=ident_b[:], in_=ident_f[:]), reads=[ident_f], writes=[ident_b])
    S.op("pool", lambda e: e.memset(ones_b[:], 1.0), writes=[ones_b])
    eps_g = S.sb("eps_g", [128, 1], F32)
    S.op("pool", lambda e: e.memset(eps_g[:], EPS), writes=[eps_g])
    modT = S.sb("modT", [128, 4, 8], F32)
    gscT = S.sb("gscT", [128, 2, 8], F32)
    ga_bc = S.sb("ga_bc", [128, 2, D], F32)
    hT = S.sb("hT", [128, 8, S_LEN], BF16)
    aff = S.sb("aff", [128, NT, NE], F32)
    posTM = S.sb("posTM", [128, NT, NE], BF16)
    idx_c = [S.sb("idx_c%d" % i, [128, 1], I32) for i in range(2 * NE)]
    gate = S.sb("gate", [128, 2 * NE], F32)
    iota_b = S.sb("iota_b", [128, 256], BF16)
    fing_bc = S.sb("fing_bc", [128, D], F32)
    load("sp", fing_bc, fing_bc[:], fing_d, fing_d.h.partition_broadcast(128))
    S.push()
    yT = S.sb("yT", [128, 8, S_LEN], BF16)
    ring = [S.sb("ring%d" % i, [128, 4096], BF16) for i in range(4)]
    ring_i = [0]

    def slot():
        r = ring[ring_i[0] % len(ring)]
        ring_i[0] += 1
        return r

    def norm_stats(src_tile, t, ss, rstd, xn_bufs, xn_dst=None):
        junk = xn_bufs["junk"]
        S.op("act", lambda e: e.activation(out=junk[:], in_=src_tile[:], func=AF.Square, accum_out=ss[:, t:t + 1]),
             reads=[src_tile], writes=[junk, (ss, t)])
        S.op("act", lambda e: e.activation(out=ss[:, t:t + 1], in_=ss[:, t:t + 1], func=AF.Ln, scale=1.0 / D,
                                           bias=eps_g[:, 0:1]), reads=[(ss, t), eps_g], writes=[(ss, t)])
        S.op("act", lambda e: e.activation(out=rstd[:, t:t + 1], in_=ss[:, t:t + 1], func=AF.Exp, scale=-0.5),
             reads=[(ss, t)], writes=[(rstd, t)])
        if xn_dst is not None:
            xtile, xkey, xap = xn_dst
            S.op("act", lambda e: e.activation(out=xap, in_=src_tile[:], func=AF.Copy, scale=rstd[:, t:t + 1]),
                 reads=[src_tile, (rstd, t)], writes=[(xtile, xkey)])
            return None
        xn = xn_bufs["xn"][t % 2]
        S.op("act", lambda e: e.activation(out=xn[:], in_=src_tile[:], func=AF.Copy, scale=rstd[:, t:t + 1]),
             reads=[src_tile, (rstd, t)], writes=[xn])
        return xn

    def transp_evac(xn, t, which, dstT, pT_bufs, tmpT, xn_src=None, act_evac=True):
        pTk = pT_bufs[t % 2]
        pT = pview(pTk, BF16, 8, 128)
        if xn_src is not None:
            xtile, xkey, xap = xn_src
            rd = (xtile, xkey)
        else:
            xap = xn[:]
            rd = xn
        for j in range(8):
            S.op("pe", lambda e, j=j: e.transpose(out=pT[:, j, :], in_=xap[:, j * 128:(j + 1) * 128],
                                                  identity=ident_b[:]), reads=[rd, ident_b], writes=[pTk],
                 sig=(j == 7))
        if t % 2 == 1 and act_evac:
            for j in range(8):
                S.op("act", lambda e, j=j: e.activation(out=dstT[:, j, t * 128:(t + 1) * 128], in_=pT[:, j, :],
                                                        func=AF.Identity, scale=gscT[:, which, j:j + 1],
                                                        bias=modT[:, 2 * which, j:j + 1]),
                     reads=[pTk, gscT, modT], writes=[(dstT, t)])
            return
        S.op("dve", lambda e: e.tensor_tensor(out=tmpT[:], in0=pT, in1=bc3(gscT[:, which, :], 128), op=ALU.mult),
             reads=[pTk, gscT], writes=[tmpT])
        S.op("dve", lambda e: e.tensor_tensor(out=dstT[:, :, t * 128:(t + 1) * 128], in0=tmpT[:],
                                              in1=bc3(modT[:, 2 * which, :], 128), op=ALU.add),
             reads=[tmpT, modT], writes=[(dstT, t)])


    S.push()
    xt = [S.sb("xt%d" % i, [128, D], F32) for i in range(3)]
    nb = dict(junk=S.sb("junk", [128, D], BF16))
    xn_all = S.sb("xn_all", [128, NT, D], BF16)
    ssB = S.sb("ssB", [128, NT], F32)
    rstdB = S.sb("rstdB", [128, NT], F32)
    tmpT = S.sb("tmpT", [128, 8, 128], F32)
    pTb = [S.ps("pTb%d" % i, [128, 512], F32) for i in range(2)]
    cT = S.sb("cT_sb", [128, 8], F32)
    scT = S.sb("scT", [128, 8], F32)
    crep = S.sb("crep", [128, 8, 128], BF16)
    b_blk = [S.sb("b_blk%d" % i, [128, 512], F32) for i in range(3)]
    mod_tmp = S.sb("mod_tmp", [128, 4, D], F32)
    g12T = S.sb("g12T", [128, 2, 8], F32)
    dtmp = S.sb("dtmp", [128, 8, 128], F32)
    load("sp", cT, cT[:], cT_d, cT_d.h)
    load("sp", g12T, g12T[:, 0, :], g1T_d, g1T_d.h)
    load("sp", g12T, g12T[:, 1, :], g2T_d, g2T_d.h)
    S.op("act", lambda e: e.activation(out=scT[:], in_=cT[:], func=AF.Silu), reads=[cT], writes=[scT])
    for k in range(8):
        S.op("dve", lambda e, k=k: e.tensor_scalar(out=crep[:, k, :], in0=ones_b[:], scalar1=scT[:, k:k + 1],
                                                  scalar2=None, op0=ALU.mult), reads=[ones_b, scT], writes=[crep])
    for t in range(NT):
        xb = xt[t % 3]
        load("sp", xb, xb[:], x_d, x_d[t * 128:(t + 1) * 128, :])
        norm_stats(xb, t, ssB, rstdB, nb, xn_dst=(xn_all, t, xn_all[:, t, :]))
    wada_v = wada_d.h.rearrange("(k p) n -> p k n", p=128)
    pA = [S.ps("pA%d" % i, [128, 512], F32) for i in range(2)]
    seg_dst = {0: ("m", 0), 1: ("m", 1), 2: ("g", 0), 3: ("m", 2), 4: ("m", 3), 5: ("g", 1)}
    def extract_mod(si):
        S.op("dve", lambda e: e.tensor_tensor(
            out=dtmp[:], in0=mod_tmp[:, si, :].rearrange("p (a b) -> p a b", a=8),
            in1=bcm(ident_f[:], 8), op=ALU.mult), reads=[(mod_tmp, si), ident_f], writes=[dtmp])
        S.op("dve", lambda e: e.tensor_reduce(out=modT[:, si, :], in_=dtmp[:], axis=AX.X, op=ALU.add),
             reads=[dtmp], writes=[modT])

    def mk_gsc(i, si):
        S.op("dve", lambda e: e.scalar_tensor_tensor(
            out=gscT[:, i, :], in0=modT[:, si, :], scalar=1.0, in1=g12T[:, i, :], op0=ALU.add, op1=ALU.mult),
            reads=[modT, g12T], writes=[gscT])

    for blk in range(12):
        w = slot()
        wv = w[:, :].rearrange("p (k n) -> p k n", k=8)
        load("pool", w, wv, wada_d, wada_v[:, :, blk * 512:(blk + 1) * 512])
        p = pA[blk % 2]
        for k in range(8):
            S.op("pe", lambda e, p=p, k=k, wv=wv: e.matmul(
                p[:], lhsT=crep[:, k, :], rhs=wv[:, k, :], start=(k == 0), stop=(k == 7)),
                reads=[crep, w], writes=[p], sig=(k == 7))
        kind, si = seg_dst[blk // 2]
        half = blk % 2
        dst = (mod_tmp[:, si, half * 512:(half + 1) * 512] if kind == "m"
               else ga_bc[:, si, half * 512:(half + 1) * 512])
        dt_ = (mod_tmp, si) if kind == "m" else ga_bc
        c0 = blk * 512
        bb = b_blk[blk % 3]
        load("sp", bb, bb[:], bada_d, bada_d.h[:, c0:c0 + 512].partition_broadcast(128))
        S.op("dve", lambda e, p=p, dst=dst, bb=bb: e.tensor_tensor(out=dst, in0=p[:], in1=bb[:], op=ALU.add),
             reads=[p, bb], writes=[dt_])
        if blk == 3:
            extract_mod(0)
            extract_mod(1)
            mk_gsc(0, 1)
            for t in range(NT):
                transp_evac(None, t, 0, hT, pTb, tmpT, xn_src=(xn_all, t, xn_all[:, t, :]))
    extract_mod(2)
    extract_mod(3)
    mk_gsc(1, 3)
    debug_dump("modT", modT, [128, 4, 8])
    debug_dump("ga_bc", ga_bc, [128, 2, D])
    debug_dump("hT", hT, [128, 8, S_LEN], BF16)
    S.pop()
    if stop_after in ("A", "B"):
        S.pop()
        return finish(nc, S, ins, dbg_out, final_reads)

    win_v = win_d.h.rearrange("(k p) n -> p k n", p=128)

    S.push()
    cos_sb = S.sb("cos_sb", [128, NT, 64], F32)
    sin_sb = S.sb("sin_sb", [128, NT, 64], F32)
    maskT_sb = S.sb("maskT_sb", [128, 4, 128], F32)
    kdec_sb = S.sb("kdec_sb", [128, 8], F32)
    qdec_sb = S.sb("qdec_sb", [128, 8, 128], F32)
    gn_bc = S.sb("gn_bc", [128, 512], F32)
    load("sp", cos_sb, cos_sb[:], cos_d, cos_d.h)
    load("sp", sin_sb, sin_sb[:], sin_d, sin_d.h)
    load("sp", maskT_sb, maskT_sb[:], maskT_d, maskT_d.h)
    load("sp", kdec_sb, kdec_sb[:], kdec_d, kdec_d.h)
    load("sp", qdec_sb, qdec_sb[:].rearrange("p a b -> p (a b)"), qdec_d, qdec_d.h)
    load("sp", gn_bc, gn_bc[:], gn_d, gn_d.h.partition_broadcast(128))
    qkTM = S.sb("qkTM", [128, NT, 2, 128], BF16)
    vTM = S.sb("vTM", [128, NT, 128], BF16)
    vFB = S.sb("vFB", [128, NT, 2, 128], BF16)
    rgs = S.sb("rgs", [128, NT, 128], BF16)
    qkT = S.sb("qkT", [128, 2, S_LEN], BF16)
    qTfb = S.sb("qTfb", [128, 2, S_LEN], BF16)
    prevFB = S.sb("prevFB", [128, 2, NT, 128], BF16)
    stFB = [S.sb("stFB%d" % i, [128, 128], F32) for i in range(2)]
    yr = S.sb("yr", [128, NT, 128], F32)
    yc = S.sb("yc", [128, NT, 128], F32)
    sqj = S.sb("sqj", [128, 128], BF16)
    yret = S.sb("yret", [128, NT, 128], BF16)
    rtc = [S.sb("rtc%d" % i, [128, 2, 64, 2], F32) for i in range(2)]
    rts = [S.sb("rts%d" % i, [128, 2, 64, 2], F32) for i in range(2)]
    qkraw = [S.sb("qkraw%d" % i, [128, 256], F32) for i in range(2)]
    sT = [S.sb("sT%d" % i, [128, 128], BF16) for i in range(2)]
    st1 = S.sb("st1", [128, NT], F32)
    st2 = S.sb("st2", [128, NT], F32)
    st3 = S.sb("st3", [128, NT], F32)
    pr = [S.ps("pr%d" % i, [128, 512], F32) for i in range(2)]
    pq = [S.ps("pq%d" % i, [128, 512], F32) for i in range(2)]
    pkv = [S.ps("pkv0", [128, 512], F32)]
    pssb = [S.ps("pss%d" % i, [128, 512], F32) for i in range(2)]
    po = S.ps("po", [128, 512], F32)
    def load_R(h):
        w = slot()
        wv = w[:, :].rearrange("p (k s c) -> p k s c", k=8, s=4)
        for s_ in range(4):
            c0 = s_ * 512 + h * 128
            load("pool", w, wv[:, :, s_, :], win_d, win_v[:, :, c0:c0 + 128])
        return w

    def load_D(h):
        w = slot()
        wq = w[:, 0:1024].rearrange("p (k n) -> p k n", k=8)
        wk = w[:, 1024:2048].rearrange("p (k n) -> p k n", k=8)
        wvv = w[:, 2048:3072].rearrange("p (k n) -> p k n", k=8)
        load("pool", w, wq[:, :, 0:64], win_d, win_v[:, :, 2048 + h * 64:2048 + (h + 1) * 64])
        load("pool", w, wq[:, :, 64:128], win_d, win_v[:, :, 2304 + h * 64:2304 + (h + 1) * 64])
        load("pool", w, wk[:, :, 0:64], win_d, win_v[:, :, 2560 + h * 64:2560 + (h + 1) * 64])
        load("pool", w, wk[:, :, 64:128], win_d, win_v[:, :, 2816 + h * 64:2816 + (h + 1) * 64])
        load("pool", w, wvv, win_d, win_v[:, :, 3072 + h * 128:3072 + (h + 1) * 128])
        return w

    wnext = load_R(0)
    for h in range(4):
        w = wnext
        wnext = load_R(h + 1) if h < 3 else load_D(0)
        conv_batch(6)
        wflat = w[:, :].rearrange("p (k n) -> p k n", k=8)

        def r_proj(t):
            p = pr[t % 2]
            for k in range(8):
                S.op("pe", lambda e, k=k: e.matmul(
                    p[:], lhsT=hT[:, k, t * 128:(t + 1) * 128], rhs=wflat[:, k, :], start=(k == 0), stop=(k == 7)),
                    reads=[(hT, t), w], writes=[p], sig=(k == 7))

        def r_evac(t):
            p = pr[t % 2]
            raw = qkraw[t % 2]
            S.op("act", lambda e: e.activation(out=raw[:], in_=p[:, 0:256], func=AF.Copy), reads=[p], writes=[raw])
            S.op("act", lambda e: e.activation(out=vTM[:, t, :], in_=p[:, 256:384], func=AF.Copy), reads=[p],
                 writes=[(vTM, t)])
            S.op("act", lambda e: e.activation(out=rgs[:, t, :], in_=p[:, 384:512], func=AF.Silu), reads=[p],
                 writes=[(rgs, t)])
            S.op("pool", lambda e: e.tensor_tensor(out=rgs[:, t, :], in0=rgs[:, t, :], in1=gn_bc[:, h * 128:(h + 1) * 128],
                                                   op=ALU.mult), reads=[(rgs, t), gn_bc], writes=[(rgs, t)])
            for dr in range(2):
                S.op("dve", lambda e, dr=dr: e.tensor_scalar(
                    out=vFB[:, t, dr, :], in0=p[:, 256:384], scalar1=kdec_sb[:, 4 * dr + h:4 * dr + h + 1],
                    scalar2=None, op0=ALU.mult), reads=[p, kdec_sb], writes=[(vFB, t)])
            rv = raw[:].rearrange("p (a i two) -> p a i two", a=2, two=2)
            cv = cos_sb[:, t, :].unsqueeze(1).unsqueeze(3).to_broadcast([128, 2, 64, 2])
            sv = sin_sb[:, t, :].unsqueeze(1).unsqueeze(3).to_broadcast([128, 2, 64, 2])
            tc_ = rtc[t % 2]
            ts_ = rts[t % 2]
            ov = qkTM[:, t, :, :].rearrange("p a (i two) -> p a i two", two=2)
            S.op("pool", lambda e: e.tensor_tensor(out=tc_[:], in0=rv, in1=cv, op=ALU.mult), reads=[raw, cos_sb],
                 writes=[tc_])
            S.op("dve", lambda e: e.tensor_tensor(out=ts_[:], in0=rv, in1=sv, op=ALU.mult), reads=[raw, sin_sb],
                 writes=[ts_])
            S.op("pool", lambda e: e.tensor_tensor(out=ov[:, :, :, 0], in0=tc_[:, :, :, 0], in1=ts_[:, :, :, 1],
                                                   op=ALU.subtract), reads=[tc_, ts_], writes=[(qkTM, (t, 0))])
            S.op("dve", lambda e: e.tensor_tensor(out=ov[:, :, :, 1], in0=tc_[:, :, :, 1], in1=ts_[:, :, :, 0],
                                                  op=ALU.add), reads=[tc_, ts_], writes=[(qkTM, (t, 1))])

        def r_T(t):
            pqk = pq[t % 2]
            pqt = pview(pqk, BF16, 2, 128)
            for a in range(2):
                S.op("pe", lambda e, a=a: e.transpose(out=pqt[:, a, :], in_=qkTM[:, t, a, :], identity=ident_b[:]),
                     reads=[(qkTM, (t, 0)), (qkTM, (t, 1)), ident_b], writes=[pqk], sig=(a == 1))

        def r_F(t):
            pqk = pq[t % 2]
            pqt = pview(pqk, BF16, 2, 128)
            S.op("act", lambda e: e.activation(out=qkT[:, :, t * 128:(t + 1) * 128], in_=pqt, func=AF.Copy),
                 reads=[pqk], writes=[(qkT, t)])
            for dr in range(2):
                S.op("dve", lambda e, dr=dr: e.tensor_tensor(
                    out=qTfb[:, dr, t * 128:(t + 1) * 128], in0=pqt[:, 0, :], in1=qdec_sb[:, 4 * dr + h, :],
                    op=ALU.mult), reads=[pqk, qdec_sb], writes=[(qTfb, t)])

        r_proj(0)
        r_evac(0)
        r_proj(1)
        for t in range(NT):
            if t + 1 < NT:
                r_evac(t + 1)
            r_T(t)
            if t + 2 < NT:
                r_proj(t + 2)
            r_F(t)
        if stop_after == 'R0a':
            debug_dump('qkTM0', qkTM, [128, NT, 2, 128], BF16)
            S.pop()
            return finish(nc, S, ins, dbg_out, final_reads)
        for dr in range(2):
            S.op("pool", lambda e, dr=dr: e.memset(stFB[dr][:], 0.0), writes=[stFB[dr]])
        kvb = [pkv[0], pq[0], pq[1]]
        kvi = 0
        orders = [list(range(NT)), list(range(NT - 1, -1, -1))]
        for i in range(NT):
            for dr in range(2):
                n = orders[dr][i]
                S.op("dve" if dr == 0 else "pool", lambda e, dr=dr, n=n: e.tensor_copy(out=prevFB[:, dr, n, :],
                                                                                     in_=stFB[dr][:]),
                     reads=[stFB[dr]], writes=[(prevFB, (dr, n))])
                if i == NT - 1:
                    continue
                pk = kvb[kvi % 3]
                kvi += 1
                S.op("pe", lambda e, pk=pk, n=n, dr=dr: e.matmul(pk[:, 0:128], lhsT=qkTM[:, n, 1, :],
                                                                 rhs=vFB[:, n, dr, :], start=True, stop=True),
                     reads=[(qkTM, (n, 0)), (qkTM, (n, 1)), (vFB, n)], writes=[pk])
                S.op("dve", lambda e, pk=pk, dr=dr: e.scalar_tensor_tensor(
                    out=stFB[dr][:], in0=stFB[dr][:], scalar=cdec[4 * dr + h], in1=pk[:, 0:128], op0=ALU.mult,
                    op1=ALU.add), reads=[stFB[dr], pk], writes=[stFB[dr]])
        if stop_after == 'R0b':
            debug_dump('prevFB', prevFB, [128, 2, NT, 128], BF16)
            S.pop()
            return finish(nc, S, ins, dbg_out, final_reads)
        def c_score(n):
            cs = slice(n * 128, (n + 1) * 128)
            pss = pssb[n % 2]
            S.op("pe", lambda e: e.matmul(pss[:, 0:128], lhsT=qkT[:, 1, cs], rhs=qkT[:, 0, cs], start=True, stop=True),
                 reads=[(qkT, n)], writes=[pss])

        def c_rest(n, nxt):
            cs = slice(n * 128, (n + 1) * 128)
            pss = pssb[n % 2]
            sTb = sT[n % 2]
            S.op("dve", lambda e: e.tensor_tensor(out=sTb[:], in0=pss[:, 0:128], in1=maskT_sb[:, h, :], op=ALU.mult),
                 reads=[pss, maskT_sb], writes=[sTb])
            if nxt is not None:
                c_score(nxt)
            S.op("pe", lambda e: e.matmul(po[:, 0:128], lhsT=sTb[:], rhs=vTM[:, n, :], start=True, stop=False),
                 reads=[sTb, (vTM, n)], writes=[po], sig=False)
            S.op("pe", lambda e: e.matmul(po[:, 0:128], lhsT=qTfb[:, 0, cs], rhs=prevFB[:, 0, n, :], start=False,
                                          stop=False), reads=[(qTfb, n), (prevFB, (0, n))], writes=[po], sig=False)
            S.op("pe", lambda e: e.matmul(po[:, 0:128], lhsT=qTfb[:, 1, cs], rhs=prevFB[:, 1, n, :], start=False,
                                          stop=True), reads=[(qTfb, n), (prevFB, (1, n))], writes=[po])
            S.op("act", lambda e: e.activation(out=yr[:, n, :], in_=po[:, 0:128], func=AF.Copy,
                                               accum_out=st1[:, n:n + 1]), reads=[po], writes=[(yr, n), (st1, n)])
            S.op("act", lambda e: e.activation(out=sqj[:], in_=yr[:, n, :], func=AF.Square, accum_out=st2[:, n:n + 1]),
                 reads=[(yr, n)], writes=[sqj, (st2, n)])

        c_score(0)
        for n in range(NT):
            c_rest(n, n + 1 if n + 1 < NT else None)
        if h == 0:
            debug_dump("yr0", yr, [128, NT, 128])
            debug_dump("qkTM0", qkTM, [128, NT, 2, 128], BF16)
        if stop_after == 'R0c':
            S.pop()
            return finish(nc, S, ins, dbg_out, final_reads)
        S.op("dve", lambda e: e.tensor_scalar(out=st1[:], in0=st1[:], scalar1=1.0 / 128, scalar2=None, op0=ALU.mult),
             reads=[st1], writes=[st1])
        S.op("dve", lambda e: e.tensor_tensor(out=st3[:], in0=st1[:], in1=st1[:], op=ALU.mult), reads=[st1],
             writes=[st3])
        S.op("dve", lambda e: e.scalar_tensor_tensor(out=st2[:], in0=st2[:], scalar=1.0 / 128, in1=st3[:],
                                                     op0=ALU.mult, op1=ALU.subtract), reads=[st2, st3], writes=[st2])
        S.op("act", lambda e: e.activation(out=st2[:], in_=st2[:], func=AF.Ln, bias=eps_g[:, 0:1]), reads=[st2, eps_g],
             writes=[st2])
        S.op("act", lambda e: e.activation(out=st3[:], in_=st2[:], func=AF.Exp, scale=-0.5), reads=[st2], writes=[st3])
        for n in range(NT):
            S.op("dve", lambda e, n=n: e.tensor_scalar(out=yc[:, n, :], in0=yr[:, n, :], scalar1=st1[:, n:n + 1],
                                                      scalar2=st3[:, n:n + 1], op0=ALU.subtract, op1=ALU.mult),
                 reads=[(yr, n), st1, st3], writes=[(yc, n)])
        S.op("dve", lambda e: e.tensor_tensor(out=yret[:], in0=yc[:], in1=rgs[:], op=ALU.mult), reads=[yc, rgs],
             writes=[yret])
        for g in range(4):
            pt4 = pr[g % 2]
            ptv = pview(pt4, BF16, 4, 128)
            for j in range(4):
                S.op("pe", lambda e, ptv=ptv, j=j, g=g: e.transpose(out=ptv[:, j, :], in_=yret[:, 4 * g + j, :],
                                                                   identity=ident_b[:]),
                     reads=[yret, ident_b], writes=[pt4])
            S.op("act", lambda e, ptv=ptv, g=g, h=h: e.activation(
                out=yT[:, h, g * 512:(g + 1) * 512].rearrange("p (a b) -> p a b", a=4), in_=ptv, func=AF.Copy),
                reads=[pt4], writes=[(yT, (h, g))])
    debug_dump("yT", yT, [128, 8, S_LEN], BF16)
    S.pop()
    if stop_after == "R":
        S.pop()
        return finish(nc, S, ins, dbg_out, final_reads)

    S.push()
    LAM_INIT = 0.8 - 0.6 * math.exp(-0.3 * 0)
    biasT_sb = S.sb("biasT_sb", [128, 4, 384], F32)
    bfar_sb = S.sb("bfar_sb", [128, 8], F32)
    lam_bc = S.sb("lam_bc", [128, 4, 64], F32)
    lam_t = S.sb("lam_t", [128, 2, 64], F32)
    lam_s = S.sb("lam_s", [128, 4], F32)
    neglam = S.sb("neglam", [128, 1], F32)
    subln_bc = S.sb("subln_bc", [128, 128], F32)
    load("sp", biasT_sb, biasT_sb[:], biasT_d, biasT_d.h)
    load("sp", bfar_sb, bfar_sb[:], bfar_d, bfar_d.h)
    load("sp", lam_bc, lam_bc[:].rearrange("p a b -> p (a b)"), lam_d, lam_d.h.partition_broadcast(128))
    load("sp", subln_bc, subln_bc[:], subln_d, subln_d.h.partition_broadcast(128))
    S.op("dve", lambda e: e.tensor_scalar(out=subln_bc[:], in0=subln_bc[:], scalar1=1.0 - LAM_INIT, scalar2=None,
                                          op0=ALU.mult), reads=[subln_bc], writes=[subln_bc])
    for i in range(2):
        S.op("dve", lambda e, i=i: e.tensor_tensor(out=lam_t[:, i, :], in0=lam_bc[:, 2 * i, :], in1=lam_bc[:, 2 * i + 1, :],
                                                  op=ALU.mult), reads=[lam_bc], writes=[lam_t])
    S.op("dve", lambda e: e.tensor_reduce(out=lam_s[:, 0:2], in_=lam_t[:], axis=AX.X, op=ALU.add), reads=[lam_t],
         writes=[lam_s])
    S.op("act", lambda e: e.activation(out=lam_s[:, 2:4], in_=lam_s[:, 0:2], func=AF.Exp), reads=[lam_s], writes=[lam_s])
    S.op("dve", lambda e: e.tensor_tensor(out=neglam[:], in0=lam_s[:, 3:4], in1=lam_s[:, 2:3], op=ALU.subtract),
         reads=[lam_s], writes=[neglam])
    S.op("dve", lambda e: e.tensor_scalar(out=neglam[:], in0=neglam[:], scalar1=-LAM_INIT, scalar2=None, op0=ALU.add),
         reads=[neglam], writes=[neglam])
    qT_d = S.sb("qT_d", [128, S_LEN], BF16)
    kT_d = S.sb("kT_d", [128, 2, S_LEN], BF16)
    S.op("pool", lambda e: e.memset(kT_d[:], 0.0), writes=[kT_d])
    v_aug = S.sb("v_aug", [128, NT, 130], BF16)
    pT_sb = [[S.sb("pT_sb%d%d" % (m, i), [128, 512], BF16) for i in range(2)] for m in range(2)]
    btmp = [S.sb("btmp%d" % i, [128, 384], F32) for i in range(2)]
    osb = [S.sb("osb%d" % m, [128, 512], F32) for m in range(2)]
    recb = [S.sb("recb%d" % m, [128, 512], F32) for m in range(2)]
    ydT = S.sb("ydT", [128, 512], F32)
    ysqT = S.sb("ysqT", [128, 512], F32)
    rstdT = S.sb("rstdT", [128, 512], F32)
    ones_f = S.sb("ones_f", [128, 128], F32)
    sublnT = S.sb("sublnT", [128, 1], F32)
    S.op("pool", lambda e: e.memset(ones_f[:], 1.0), writes=[ones_f])
    eps_c = S.sb("eps_c", [128, 1], F32)
    S.op("pool", lambda e: e.memset(eps_c[:], EPS), writes=[eps_c])
    with nc.allow_non_contiguous_dma(reason="tiny 128-element column load"):
        load("sp", sublnT, sublnT[:], subln_d, subln_d.h.rearrange("o d -> d o"))
    S.op("dve", lambda e: e.tensor_scalar(out=sublnT[:], in0=sublnT[:], scalar1=1.0 - LAM_INIT, scalar2=None,
                                          op0=ALU.mult), reads=[sublnT], writes=[sublnT])
    scb = [S.ps("scb%d" % m, [128, 512], F32) for m in range(3)]
    Ob = [S.ps("Ob%d" % i, [128, 512], F32) for i in range(2)]
    Rb = [S.ps("Rb%d" % i, [128, 512], F32) for i in range(2)]
    pm = S.ps("pm", [128, 512], F32)
    prb = pm

    btc = [0]
    wGpre = None
    pend_norm = []
    for h in range(4):
        w = wnext
        wq = w[:, 0:1024].rearrange("p (k n) -> p k n", k=8)
        wk = w[:, 1024:2048].rearrange("p (k n) -> p k n", k=8)
        wvv = w[:, 2048:3072].rearrange("p (k n) -> p k n", k=8)
        if h < 3:
            wnext = load_D(h + 1)
        else:
            wRO = slot()
            wDO = slot()
            wROv = wRO[:, :].rearrange("p (f n) -> p f n", f=4)
            wDOv = wDO[:, :].rearrange("p (f n) -> p f n", f=4)
            load("pool", wRO, wROv, wro_d, wro_d.h.rearrange("(f p) n -> p f n", p=128))
            load("pool", wDO, wDOv, wdo_d, wdo_d.h.rearrange("(f p) n -> p f n", p=128))
        conv_batch(6)
        pjb = [pm, scb[0], scb[1], scb[2]]
        pji = 0
        for (wx, isq) in ((wq, True), (wk, False)):
            for c in range(4):
                pb = pjb[pji % 4]
                pji += 1
                for k in range(8):
                    S.op("pe", lambda e, wx=wx, k=k, c=c, pb=pb: e.matmul(pb[:], lhsT=wx[:, k, :],
                                                                         rhs=hT[:, k, c * 512:(c + 1) * 512],
                                                                         start=(k == 0), stop=(k == 7)),
                         reads=[w] + [(hT, 4 * c + i) for i in range(4)], writes=[pb], sig=(k == 7))
                if isq:
                    S.op("act", lambda e, c=c, pb=pb: e.activation(out=qT_d[:, c * 512:(c + 1) * 512], in_=pb[:],
                                                                   func=AF.Copy, scale=0.125), reads=[pb],
                         writes=[(qT_d, c)])
                else:
                    for m in range(2):
                        S.op("dve" if m == 0 else "act", lambda e, c=c, m=m, pb=pb: (
                            e.tensor_copy(out=kT_d[m * 64:(m + 1) * 64, m, c * 512:(c + 1) * 512],
                                          in_=pb[m * 64:(m + 1) * 64, :]) if m == 0 else
                            e.activation(out=kT_d[m * 64:(m + 1) * 64, m, c * 512:(c + 1) * 512],
                                         in_=pb[m * 64:(m + 1) * 64, :], func=AF.Copy)), reads=[pb],
                            writes=[(kT_d, (c, m))])
        for g in range(4):
            pb = pjb[pji % 4]
            pji += 1
            for j in range(4):
                t = 4 * g + j
                for k in range(8):
                    S.op("pe", lambda e, k=k, t=t, j=j, pb=pb: e.matmul(pb[:, j * 128:(j + 1) * 128],
                                                                       lhsT=hT[:, k, t * 128:(t + 1) * 128],
                                                                       rhs=wvv[:, k, :], start=(k == 0), stop=(k == 7)),
                         reads=[w, (hT, t)], writes=[pb], sig=(k == 7 and j == 3))
            S.op("act", lambda e, g=g, pb=pb: e.activation(out=v_aug[:, 4 * g:4 * g + 4, 0:128],
                                                           in_=pb[:, :].rearrange("p (a b) -> p a b", a=4), func=AF.Copy),
                 reads=[pb], writes=[(v_aug, g)])
        def qk_op(c, kb, m):
            sc = scb[(2 * kb + m) % 3]
            S.op("pe", lambda e: e.matmul(
                sc[:], lhsT=kT_d[:, m, kb * 128:(kb + 1) * 128],
                rhs=qT_d[:, c * 512:(c + 1) * 512], start=True, stop=True),
                reads=[(kT_d, (kb // 4, m)), (qT_d, c)], writes=[sc])

        def exp_op(c, kb, m):
            sc = scb[(2 * kb + m) % 3]
            pT = pT_sb[m][kb % 2]
            blocks = list(range(4 * c, 4 * c + 4))
            far_r = [g for g in blocks if g < kb - 1]
            near = [g for g in blocks if abs(g - kb) <= 1]
            far_l = [g for g in blocks if g > kb + 1]
            bt = None
            if near:
                c0 = (near[0] - 4 * c) * 128
                c1 = (near[-1] - 4 * c + 1) * 128
                j0 = (near[0] - kb + 1) * 128
                bt = btmp[btc[0] % 2]
                btc[0] += 1
                S.op("dve", lambda e: e.tensor_tensor(
                    out=bt[:, 0:c1 - c0], in0=sc[:, c0:c1], in1=biasT_sb[:, h, j0:j0 + c1 - c0], op=ALU.add),
                    reads=[sc, biasT_sb], writes=[bt])
            for (reg, bi) in ((far_r, h), (far_l, 4 + h)):
                if reg:
                    r0 = (reg[0] - 4 * c) * 128
                    r1 = (reg[-1] - 4 * c + 1) * 128
                    S.op("act", lambda e, r0=r0, r1=r1, bi=bi: e.activation(
                        out=pT[:, r0:r1], in_=sc[:, r0:r1], func=AF.Exp, bias=bfar_sb[:, bi:bi + 1], scale=1.0),
                        reads=[sc, bfar_sb], writes=[pT])
            if near:
                S.op("act", lambda e: e.activation(out=pT[:, c0:c1], in_=bt[:, 0:c1 - c0], func=AF.Exp),
                     reads=[bt], writes=[pT])

        def pv_op(c, kb, m):
            pT = pT_sb[m][kb % 2]
            S.op("pe", lambda e: e.matmul(Ob[m][:], lhsT=v_aug[:, kb, 0:128], rhs=pT[:], start=(kb == 0),
                                          stop=(kb == NT - 1)), reads=[pT, (v_aug, kb // 4)], writes=[Ob[m]])
            S.op("pe", lambda e: e.matmul(Rb[m][:], lhsT=ones_b[:], rhs=pT[:], start=(kb == 0),
                                          stop=(kb == NT - 1)), reads=[pT, ones_b], writes=[Rb[m]])

        def norm_a(c):
            for m in range(2):
                S.op("dve", lambda e, m=m: e.tensor_copy(out=osb[m][:], in_=Ob[m][:]), reads=[Ob[m]], writes=[osb[m]])
                S.op("act", lambda e, m=m: e.activation(out=recb[m][:], in_=Rb[m][:], func=AF.Ln), reads=[Rb[m]],
                     writes=[recb[m]])
            for m in range(2):
                S.op("act", lambda e, m=m: e.activation(out=recb[m][:], in_=recb[m][:], func=AF.Exp, scale=-1.0),
                     reads=[recb[m]], writes=[recb[m]])
                S.op("dve", lambda e, m=m: e.tensor_tensor(out=osb[m][:], in0=osb[m][:], in1=recb[m][:], op=ALU.mult),
                     reads=[osb[m], recb[m]], writes=[osb[m]])
            S.op("dve", lambda e: e.scalar_tensor_tensor(out=ydT[:], in0=osb[1][:], scalar=neglam[:, 0:1], in1=osb[0][:],
                                                         op0=ALU.mult, op1=ALU.add), reads=[osb[0], osb[1], neglam],
                 writes=[ydT])
            S.op("dve", lambda e: e.tensor_tensor(out=ysqT[:], in0=ydT[:], in1=ydT[:], op=ALU.mult), reads=[ydT],
                 writes=[ysqT])

        def norm_b(hh, c):
            S.op("pe", lambda e: e.matmul(prb[:], lhsT=ones_f[:], rhs=ysqT[:], start=True, stop=True),
                 reads=[ones_f, ysqT], writes=[prb])
            S.op("act", lambda e: e.activation(out=rstdT[:], in_=prb[:], func=AF.Ln, scale=1.0 / 128, bias=eps_c[:, 0:1]),
                 reads=[prb, eps_c], writes=[rstdT])
            S.op("act", lambda e: e.activation(out=rstdT[:], in_=rstdT[:], func=AF.Exp, scale=-0.5), reads=[rstdT],
                 writes=[rstdT])
            S.op("dve", lambda e: e.scalar_tensor_tensor(
                out=yT[:, 4 + hh, c * 512:(c + 1) * 512], in0=ydT[:], scalar=sublnT[:, 0:1], in1=rstdT[:], op0=ALU.mult,
                op1=ALU.mult), reads=[ydT, sublnT, rstdT], writes=[(yT, (4 + hh, c))])

        for c in range(4):
            its = [(kb, m) for kb in range(NT) for m in range(2)]
            LOOK = 2
            for i in range(min(LOOK, len(its))):
                qk_op(c, *its[i])
            for i, (kb, m) in enumerate(its):
                exp_op(c, kb, m)
                if i + LOOK < len(its):
                    qk_op(c, *its[i + LOOK])
                pv_op(c, kb, m)
                if i == 8 and pend_norm:
                    norm_b(*pend_norm.pop())
            norm_a(c)
            pend_norm.append((h, c))
    while pend_norm:
        norm_b(*pend_norm.pop())
    debug_dump("yTd", yT, [128, 8, S_LEN], BF16)
    S.pop()
    if stop_after == "D":
        S.pop()
        return finish(nc, S, ins, dbg_out, final_reads)

    S.push()
    mergedT = S.sb("mergedT", [128, 8, S_LEN], BF16)
    conv_batch(3 * NE)
    pG = [[S.ps("pG%d%d" % (a, i), [128, 512], F32) for i in range(2)] for a in range(4)]
    wO = S.sb("wO", [128, 8, D], BF16)
    load("pool", wO, wO[:], wo_d, wo_d.h.rearrange("(k p) n -> p k n", p=128))
    wr = S.sb("wr", [128, 8, NE], BF16)
    load("pool", wr, wr[:], wr_d, wr_d.h.rearrange("(k p) n -> p k n", p=128))
    S.push()
    wGs = [S.sb("wG%d" % i, [128, 8, 2, 128], BF16) for i in range(2)]
    sg = [S.sb("sg%d" % i, [128, 512], F32) for i in range(2)]
    m1 = S.sb("m1", [128, 512], F32)
    m2 = S.sb("m2", [128, 512], F32)
    it = 0
    for j in range(8):
        wG = wGs[j % 2]
        load("pool", wG, wG[:, :, 0, :], win_d, win_v[:, :, 3584 + j * 128:3584 + (j + 1) * 128])
        load("pool", wG, wG[:, :, 1, :], win_d, win_v[:, :, 4608 + j * 128:4608 + (j + 1) * 128])
        for c in range(4):
            b = it % 2
            it += 1
            pg, pgd, pR, pDd = pG[0][b], pG[1][b], pG[2][b], pG[3][b]
            csl = slice(c * 512, (c + 1) * 512)
            hreads = [(hT, 4 * c + i) for i in range(4)]
            for gi, pp in ((0, pg), (1, pgd)):
                for k in range(8):
                    S.op("pe", lambda e, pp=pp, gi=gi, k=k, wG=wG, csl=csl: e.matmul(
                        pp[:], lhsT=wG[:, k, gi, :], rhs=hT[:, k, csl], start=(k == 0), stop=(k == 7)),
                        reads=[wG] + hreads, writes=[pp], sig=(k == 7))
            for (pp, wv_, base) in ((pR, wROv, 0), (pDd, wDOv, 4)):
                for f in range(4):
                    S.op("pe", lambda e, pp=pp, wv_=wv_, base=base, f=f, j=j, csl=csl: e.matmul(
                        pp[:], lhsT=wv_[:, f, j * 128:(j + 1) * 128], rhs=yT[:, base + f, csl], start=(f == 0),
                        stop=(f == 3)), reads=[wRO, wDO, yT], writes=[pp], sig=(f == 3))
            S.op("act", lambda e, pg=pg: e.activation(out=sg[0][:], in_=pg[:], func=AF.Sigmoid), reads=[pg], writes=[sg[0]])
            S.op("act", lambda e, pgd=pgd: e.activation(out=sg[1][:], in_=pgd[:], func=AF.Sigmoid), reads=[pgd],
                 writes=[sg[1]])
            S.op("dve", lambda e, pR=pR: e.tensor_tensor(out=m1[:], in0=pR[:], in1=sg[0][:], op=ALU.mult),
                 reads=[pR, sg[0]], writes=[m1])
            S.op("dve", lambda e, pDd=pDd: e.tensor_tensor(out=m2[:], in0=pDd[:], in1=sg[1][:], op=ALU.mult),
                 reads=[pDd, sg[1]], writes=[m2])
            S.op("dve", lambda e, j=j, csl=csl: e.tensor_tensor(out=mergedT[:, j, csl], in0=m1[:], in1=m2[:], op=ALU.add),
                 reads=[m1, m2], writes=[(mergedT, (j, c))])
    debug_dump("mergedT", mergedT, [128, 8, S_LEN], BF16)
    S.pop()
    if stop_after == "G":
        S.pop()
        S.pop()
        return finish(nc, S, ins, dbg_out, final_reads)

    xt2 = [S.sb("xt2_%d" % i, [128, D], F32) for i in range(2)]
    x1 = [S.sb("x1_%d" % i, [128, D], F32) for i in range(3)]
    nb2 = dict(junk=S.sb("junk2", [128, D], BF16), xn=[S.sb("xn2_%d" % i, [128, D], BF16) for i in range(2)])
    ss2 = S.sb("ss2", [128, NT], F32)
    rstd2 = S.sb("rstd2", [128, NT], F32)
    tmpT2 = S.sb("tmpT2", [128, 8, 128], F32)
    esb = [S.sb("esb%d" % i, [128, NE], F32) for i in range(2)]
    rs = [S.sb("rs%d" % i, [128, 2], F32) for i in range(2)]
    pW = pG[0]
    pT2 = pG[1]
    pL = pG[2][0]
    xns = {}

    def w_s1(t):
        xb = xt2[t % 2]
        x1b = x1[t % 3]
        tsl = slice(t * 128, (t + 1) * 128)
        load("sp", xb, xb[:], x_d, x_d[tsl, :])
        for half in range(2):
            pw = pW[half]
            hs = slice(half * 512, (half + 1) * 512)
            for k in range(8):
                S.op("pe", lambda e, pw=pw, k=k, hs=hs: e.matmul(pw[:], lhsT=mergedT[:, k, tsl], rhs=wO[:, k, hs],
                                                                start=(k == 0), stop=(k == 7)),
                     reads=[mergedT, wO], writes=[pw], sig=(k == 7))
            S.op("dve", lambda e, pw=pw, hs=hs: e.tensor_tensor(out=x1b[:, hs], in0=pw[:], in1=ga_bc[:, 0, hs],
                                                               op=ALU.mult), reads=[pw, ga_bc], writes=[x1b])
            S.op("pool", lambda e, hs=hs: e.tensor_tensor(out=x1b[:, hs], in0=x1b[:, hs], in1=xb[:, hs], op=ALU.add),
                 reads=[x1b, xb], writes=[x1b])
        S.op("sp", lambda e: e.dma_start(out=acc_d[tsl, :], in_=x1b[:]), reads=[x1b], writes=[(acc_d, t)], dma=True)

    def w_s2(t):
        x1b = x1[t % 3]
        tsl = slice(t * 128, (t + 1) * 128)
        xn = norm_stats(x1b, t, ss2, rstd2, nb2)
        S.op("sp", lambda e: e.dma_start(out=xn2_d[tsl, :], in_=xn[:]), reads=[xn], writes=[(xn2_d, t)], dma=True)
        xns[t] = xn

    def w_s3(t):
        tsl = slice(t * 128, (t + 1) * 128)
        transp_evac(xns[t], t, 1, hT, pT2, tmpT2, act_evac=False)
        for k in range(8):
            S.op("pe", lambda e, k=k: e.matmul(pL[:, 0:NE], lhsT=hT[:, k, tsl], rhs=wr[:, k, :], start=(k == 0),
                                               stop=(k == 7)), reads=[(hT, t), wr], writes=[pL], sig=(k == 7))
        es_ = esb[t % 2]
        rs_ = rs[t % 2]
        S.op("act", lambda e: e.activation(out=es_[:], in_=pL[:, 0:NE], func=AF.Exp, accum_out=rs_[:, 0:1]),
             reads=[pL], writes=[es_, rs_])
        S.op("dve", lambda e: e.reciprocal(out=rs_[:, 1:2], in_=rs_[:, 0:1]), reads=[rs_], writes=[rs_])
        S.op("dve", lambda e: e.tensor_scalar(out=aff[:, t, :], in0=es_[:], scalar1=rs_[:, 1:2], scalar2=None,
                                              op0=ALU.mult), reads=[es_, rs_], writes=[(aff, t)])

    w_s1(0)
    w_s1(1)
    w_s2(0)
    for t in range(NT):
        if t + 2 < NT:
            w_s1(t + 2)
        if t + 1 < NT:
            w_s2(t + 1)
        w_s3(t)
    debug_dump("aff", aff, [128, NT, NE])
    debug_dump("h2T", hT, [128, 8, S_LEN], BF16)
    S.pop()
    if stop_after == "W":
        S.pop()
        return finish(nc, S, ins, dbg_out, final_reads)

    S.pop()
    S.push()
    wgt = [[S.sb("wE%d_%d" % (a, i), [128, 8, D], BF16) for i in range(2)] for a in range(3)]

    def load_w(e_):
        for a in range(3):
            wt = wgt[a][e_ % 2]
            S.op("sp", lambda e, wt=wt, a=a, e_=e_: e.dma_start(
                out=wt[:].rearrange("p k n -> p (k n)"), in_=wb_d[a].h[e_]), reads=[(wb_d[a], e_)], writes=[wt],
                dma=True)

    load_w(0)
    load_w(1)
    S.push()
    affT = S.sb("affT", [NE, S_LEN], F32)
    junkK = S.sb("junkK", [NE, S_LEN], F32)
    maskf = S.sb("maskf", [NE, S_LEN], F32)
    posf = S.sb("posf", [NE, S_LEN], F32)
    thr = S.sb("thr", [NE, 4], F32)
    pK = [S.ps("pK%d" % i, [128, 512], F32) for i in range(4)]
    for g in range(4):
        pk = pK[g]
        for j in range(4):
            t = 4 * g + j
            S.op("pe", lambda e, pk=pk, j=j, t=t: e.transpose(out=pk[0:NE, j * 128:(j + 1) * 128], in_=aff[:, t, :],
                                                              identity=ident_f[:]), reads=[(aff, t), ident_f],
                 writes=[pk], sig=(j == 3))
        S.op("act", lambda e, pk=pk, g=g: e.activation(out=affT[:, g * 512:(g + 1) * 512], in_=pk[0:NE, :], func=AF.Copy),
             reads=[pk], writes=[affT])
    NIT = 26
    S.op("dve", lambda e: e.memset(thr[:, 1:2], 0.5), writes=[thr])
    for itn in range(NIT):
        step = 0.5 ** (itn + 1)
        S.op("dve", lambda e: e.tensor_scalar(out=junkK[:], in0=affT[:], scalar1=thr[:, 1:2], scalar2=0.0, op0=ALU.is_ge,
                                              op1=ALU.add, accum_out=thr[:, 2:3]), reads=[affT, thr], writes=[junkK, thr])
        if itn < NIT - 1:
            S.op("dve", lambda e, step=step: e.tensor_scalar(out=thr[:, 3:4], in0=thr[:, 2:3], scalar1=float(CAP),
                                                            scalar2=step, op0=ALU.is_ge, op1=ALU.mult), reads=[thr],
                 writes=[thr])
            S.op("dve", lambda e, step=step: e.scalar_tensor_tensor(out=thr[:, 1:2], in0=thr[:, 1:2], scalar=-0.5 * step,
                                                                   in1=thr[:, 3:4], op0=ALU.add, op1=ALU.add),
                 reads=[thr], writes=[thr])
        else:
            S.op("dve", lambda e, step=step: e.tensor_scalar(out=thr[:, 3:4], in0=thr[:, 2:3], scalar1=float(CAP),
                                                            scalar2=step, op0=ALU.is_lt, op1=ALU.mult), reads=[thr],
                 writes=[thr])
            S.op("dve", lambda e: e.tensor_tensor(out=thr[:, 0:1], in0=thr[:, 1:2], in1=thr[:, 3:4], op=ALU.subtract),
                 reads=[thr], writes=[thr])
    S.op("dve", lambda e: e.tensor_scalar(out=maskf[:], in0=affT[:], scalar1=thr[:, 0:1], scalar2=None, op0=ALU.is_ge),
         reads=[affT, thr], writes=[maskf])
    S.op("dve", lambda e: e.memset(junkK[:], 1.0), writes=[junkK])
    S.op("dve", lambda e: e.tensor_tensor_scan(out=posf[:], data0=junkK[:], data1=maskf[:], initial=0.0, op0=ALU.mult,
                                               op1=ALU.add), reads=[junkK, maskf], writes=[posf])
    S.op("dve", lambda e: e.tensor_tensor(out=posf[:], in0=posf[:], in1=maskf[:], op=ALU.mult), reads=[posf, maskf],
         writes=[posf])
    S.op("dve", lambda e: e.tensor_scalar(out=posf[:], in0=posf[:], scalar1=-1.0, scalar2=None, op0=ALU.add), reads=[posf],
         writes=[posf])
    debug_dump("posf", posf, [NE, S_LEN])
    pk = pK[0]
    for t in range(NT):
        S.op("pe", lambda e, t=t: e.transpose(out=pk[:, t * NE:(t + 1) * NE], in_=posf[:, t * 128:(t + 1) * 128],
                                              identity=ident_f[0:NE, 0:NE]), reads=[posf, ident_f], writes=[pk],
             sig=(t == NT - 1))
    S.op("act", lambda e: e.activation(out=posTM[:].rearrange("p a b -> p (a b)"), in_=pk[:, 0:NT * NE], func=AF.Copy),
         reads=[pk], writes=[posTM])
    S.pop()

    iota_f = S.sb("iota_f", [128, 256], F32)
    pt_sb = S.sb("pt_sb", [128, NT, 2], F32)
    load("sp", iota_f, iota_f[:], iota_d, iota_d.h)
    load("sp", pt_sb, pt_sb[:], pt_d, pt_d.h)
    S.op("dve", lambda e: e.tensor_copy(out=iota_b[:], in_=iota_f[:]), reads=[iota_f], writes=[iota_b])
    Rr = S.sb("Rr", [128, NT, NE, 4], BF16)
    tmpf = S.sb("tmpf", [128, NT, NE], F32)
    S.op("dve", lambda e: e.tensor_copy(out=Rr[:, :, :, 0:2], in_=pt_sb[:].unsqueeze(2).to_broadcast([128, NT, NE, 2])),
         reads=[pt_sb], writes=[Rr])
    S.op("dve", lambda e: e.tensor_copy(out=Rr[:, :, :, 2], in_=aff[:]), reads=[aff], writes=[Rr])
    S.op("dve", lambda e: e.tensor_tensor(out=tmpf[:], in0=aff[:], in1=Rr[:, :, :, 2], op=ALU.subtract), reads=[aff, Rr],
         writes=[tmpf])
    S.op("dve", lambda e: e.tensor_copy(out=Rr[:, :, :, 3], in_=tmpf[:]), reads=[tmpf], writes=[Rr])
    oh = [S.sb("oh%d" % i, [128, NT, 256], BF16) for i in range(2)]
    pix = S.sb("pix", [128, 2, 4], F32)
    idxf = S.sb("idxf", [128, 2 * NE], F32)
    xin = [[S.sb("xin%d_%d" % (i, j), [128, D], BF16) for j in range(2)] for i in range(2)]
    xinT = [S.sb("xinT%d" % i, [128, 8, 256], BF16) for i in range(2)]
    hidT = S.sb("hidT", [128, 8, 256], BF16)
    sa = [S.sb("sa%d" % i, [128, 256], F32) for i in range(2)]
    ysb = [[S.sb("ysb%d_%d" % (i, j), [128, D], F32) for j in range(2)] for i in range(2)]
    tmpT3 = S.sb("tmpT3", [128, 8, 128], F32)
    pI0 = S.ps("pI0", [128, 512], F32)
    pI = [pI0, pI0]
    pYb = [S.ps("pY%d" % i, [128, 512], F32) for i in range(2)]
    pX = S.ps("pX", [128, 512], F32)
    pAU = [[S.ps("pAU%d%d" % (a, i), [128, 512], F32) for i in range(2)] for a in range(2)]
    pY = pYb
    wsrc = (wg_d, wu_d, wd_d)

    bcreg = nc.gpsimd.alloc_register("bcreg")
    nc.gpsimd.reg_mov(bcreg, S_LEN - 1)

    def gather(e_):
        for s2 in range(2):
            col = e_ * 2 + s2
            S.op("pool", lambda e, e_=e_, s2=s2, col=col: e.indirect_dma_start(
                out=xin[e_ % 2][s2][:], out_offset=None, in_=xn2_d.h,
                in_offset=bass.IndirectOffsetOnAxis(ap=idx_c[col][:, 0:1], axis=0), bounds_check=bcreg,
                oob_is_err=False), reads=[idx_c[col], xn2_d], writes=[xin[e_ % 2][s2]], dma=True)

    def scatter(e_):
        for s2 in range(2):
            col = e_ * 2 + s2
            S.op("pool", lambda e, e_=e_, s2=s2, col=col: e.indirect_dma_start(
                out=acc_d.h, out_offset=bass.IndirectOffsetOnAxis(ap=idx_c[col][:, 0:1], axis=0),
                in_=ysb[e_ % 2][s2][:], in_offset=None, bounds_check=bcreg, oob_is_err=True,
                compute_op=ALU.add), reads=[idx_c[col], ysb[e_ % 2][s2]], writes=[acc_d], dma=True)

    def bi_onehot(e_):
        ohe = oh[e_ % 2]
        S.op("dve", lambda e: e.tensor_tensor(out=ohe[:], in0=bcm(iota_b[:], NT), in1=bc3(posTM[:, :, e_], 256),
                                              op=ALU.is_equal), reads=[iota_b, posTM], writes=[ohe])

    def bi_mm(e_):
        ohe = oh[e_ % 2]
        for s2 in range(2):
            pi = pI[s2]
            for t in range(NT):
                S.op("pe", lambda e, pi=pi, t=t, s2=s2: e.matmul(
                    pi[:, 0:4], lhsT=ohe[:, t, s2 * 128:(s2 + 1) * 128], rhs=Rr[:, t, e_, :], start=(t == 0),
                    stop=(t == NT - 1)), reads=[ohe, Rr], writes=[pi], sig=(t == NT - 1))
            S.op("act", lambda e, pi=pi, s2=s2: e.activation(out=pix[:, s2, :], in_=pi[:, 0:4], func=AF.Copy), reads=[pi],
                 writes=[(pix, s2)])
            col = e_ * 2 + s2
            S.op("dve", lambda e, s2=s2, col=col: e.scalar_tensor_tensor(
                out=idxf[:, col:col + 1], in0=pix[:, s2, 1:2], scalar=128.0, in1=pix[:, s2, 0:1], op0=ALU.mult,
                op1=ALU.add), reads=[(pix, s2)], writes=[(idxf, col)])
            S.op("dve", lambda e, s2=s2, col=col: e.tensor_tensor(out=gate[:, col:col + 1], in0=pix[:, s2, 2:3],
                                                                 in1=pix[:, s2, 3:4], op=ALU.add), reads=[(pix, s2)],
                 writes=[(gate, col)])
            S.op("dve", lambda e, col=col: e.tensor_copy(out=idx_c[col][:], in_=idxf[:, col:col + 1]),
                 reads=[(idxf, col)], writes=[idx_c[col]])

    def build_index(e_):
        bi_onehot(e_)
        bi_mm(e_)

    tmpT3b = [tmpT3, tmpT3]
    pXb = [pX, pI0]

    def prep_xT(e_):
        b = e_ % 2
        xT = xinT[b]
        for s2 in range(2):
            pxk = pXb[s2]
            pxv = pview(pxk, BF16, 8, 128)
            tt = tmpT3b[s2]
            for j in range(8):
                S.op("pe", lambda e, pxv=pxv, j=j, s2=s2: e.transpose(out=pxv[:, j, :],
                                                                    in_=xin[b][s2][:, j * 128:(j + 1) * 128],
                                                                    identity=ident_b[:]),
                     reads=[xin[b][s2], ident_b], writes=[pxk], sig=(j == 7))
            S.op("dve", lambda e, pxv=pxv, tt=tt: e.tensor_tensor(out=tt[:], in0=pxv, in1=bc3(gscT[:, 1, :], 128),
                                                                 op=ALU.mult), reads=[pxk, gscT], writes=[tt])
            S.op("dve", lambda e, s2=s2, tt=tt: e.tensor_tensor(out=xT[:, :, s2 * 128:(s2 + 1) * 128], in0=tt[:],
                                                               in1=bc3(modT[:, 2, :], 128), op=ALU.add),
                 reads=[tt, modT], writes=[(xT, s2)])

    build_index(0)
    gather(0)
    build_index(1)
    gather(1)
    prep_xT(0)
    for e_ in range(NE):
        b = e_ % 2
        wg_, wu_, wd_ = wgt[0][b], wgt[1][b], wgt[2][b]
        xT = xinT[b]
        if e_ + 2 < NE:
            bi_onehot(e_ + 2)
        for fc in range(8):
            pa = pAU[0][fc % 2]
            pu = pAU[1][fc % 2]
            fs = slice(fc * 128, (fc + 1) * 128)
            for (pp, wt) in ((pa, wg_), (pu, wu_)):
                for k in range(8):
                    S.op("pe", lambda e, pp=pp, wt=wt, k=k, fs=fs, xT=xT: e.matmul(pp[:, 0:256], lhsT=wt[:, k, fs],
                                                                                rhs=xT[:, k, :], start=(k == 0),
                                                                                stop=(k == 7)),
                         reads=[wt, xT], writes=[pp], sig=(k == 7))
            sab = sa[fc % 2]
            S.op("act", lambda e, pa=pa, sab=sab: e.activation(out=sab[:], in_=pa[:, 0:256], func=AF.Silu), reads=[pa],
                 writes=[sab])
            S.op("dve", lambda e, pu=pu, sab=sab, fc=fc: e.tensor_tensor(out=hidT[:, fc, :], in0=pu[:, 0:256], in1=sab[:],
                                                                        op=ALU.mult), reads=[pu, sab],
                 writes=[(hidT, fc)])
        if e_ + 2 < NE:
            bi_mm(e_ + 2)
        if e_ + 1 < NE:
            prep_xT(e_ + 1)
        yb = ysb[b]
        for s2 in range(2):
            for dh in range(2):
                py = pY[(s2 * 2 + dh) % 2]
                ds = slice(dh * 512, (dh + 1) * 512)
                for fc in range(8):
                    S.op("pe", lambda e, py=py, fc=fc, s2=s2, ds=ds, wd_=wd_: e.matmul(
                        py[:], lhsT=hidT[:, fc, s2 * 128:(s2 + 1) * 128], rhs=wd_[:, fc, ds], start=(fc == 0),
                        stop=(fc == 7)), reads=[hidT, wd_], writes=[py], sig=(fc == 7))
                col = e_ * 2 + s2
                S.op("dve", lambda e, py=py, yb=yb, s2=s2, ds=ds, col=col: e.scalar_tensor_tensor(
                    out=yb[s2][:, ds], in0=py[:], scalar=gate[:, col:col + 1], in1=ga_bc[:, 1, ds], op0=ALU.mult,
                    op1=ALU.mult), reads=[py, (gate, col), ga_bc], writes=[yb[s2]])
        scatter(e_)
        if e_ + 2 < NE:
            load_w(e_ + 2)
            gather(e_ + 2)
    debug_dump("idxf", idxf, [128, 2 * NE])
    debug_dump("gate", gate, [128, 2 * NE])
    S.pop()

    S.push()
    xf = [S.sb("xf%d" % i, [128, D], F32) for i in range(4)]
    of_ = [S.sb("of%d" % i, [128, D], F32) for i in range(3)]
    junkF = S.sb("junkF", [128, D], BF16)
    ssF = S.sb("ssF", [128, NT], F32)
    rsF = S.sb("rsF", [128, NT], F32)
    def fload(t):
        xb = xf[t % 4]
        tsl = slice(t * 128, (t + 1) * 128)
        S.op("sp", lambda e: e.dma_start(out=xb[:], in_=acc_d[tsl, :]), reads=[acc_d], writes=[xb], dma=True)

    for t in range(4):
        fload(t)
    for t in range(NT):
        xb = xf[t % 4]
        ob = of_[t % 3]
        tsl = slice(t * 128, (t + 1) * 128)
        S.op("act", lambda e, xb=xb, t=t: e.activation(out=junkF[:], in_=xb[:], func=AF.Square, accum_out=ssF[:, t:t + 1]),
             reads=[xb], writes=[junkF, (ssF, t)])
        S.op("act", lambda e, t=t: e.activation(out=ssF[:, t:t + 1], in_=ssF[:, t:t + 1], func=AF.Ln, scale=1.0 / D,
                                                bias=eps_g[:, 0:1]), reads=[(ssF, t), eps_g], writes=[(ssF, t)])
        S.op("act", lambda e, t=t: e.activation(out=rsF[:, t:t + 1], in_=ssF[:, t:t + 1], func=AF.Exp, scale=-0.5),
             reads=[(ssF, t)], writes=[(rsF, t)])
        S.op("dve", lambda e, xb=xb, ob=ob, t=t: e.scalar_tensor_tensor(out=ob[:], in0=xb[:], scalar=rsF[:, t:t + 1],
                                                                       in1=fing_bc[:], op0=ALU.mult, op1=ALU.mult),
             reads=[xb, (rsF, t), fing_bc], writes=[ob])
        S.op("act", lambda e, ob=ob, tsl=tsl: e.dma_start(out=out_d[tsl, :], in_=ob[:]), reads=[ob], writes=[(out_d, t)],
             dma=True)
        if t + 4 < NT:
            fload(t + 4)
    S.pop()

    return finish(nc, S, ins, dbg_out, final_reads)


def finish(nc, S, ins, dbg_out, final_reads):
    S.op("sp", lambda e: e.nop(), reads=final_reads)
    S.emit()
    S.stack.close()
    return nc, dbg_out


_TABS = None


def _prep_inputs(inputs, tabs):
    f32 = np.float32
    g = lambda k: np.ascontiguousarray(np.asarray(inputs[k], dtype=f32))
    x = g("x")
    c = g("c")
    B = x.shape[0]
    rel_bias = g("rel_bias")
    biasT = np.ascontiguousarray(rel_bias[tabs["bidx"]].transpose(0, 3, 1, 2).reshape(128, 4, 384))
    bfar = np.zeros((128, 8), f32)
    bfar[:, 0:4] = rel_bias[31][None, :]
    bfar[:, 4:8] = rel_bias[15][None, :]
    lam4 = np.concatenate([g("lambda_q1")[0], g("lambda_k1")[0], g("lambda_q2")[0], g("lambda_k2")[0]])[None, :]
    shared = {
        "w_ada": g("w_ada")[0], "b_ada": g("b_ada"),
        "g1T": np.ascontiguousarray(g("norm_mix_g")[0].reshape(8, 128).T),
        "g2T": np.ascontiguousarray(g("norm_ffn_g")[0].reshape(8, 128).T),
        "final_g": g("final_g")[None, :], "w_in": g("w_in")[0],
        "ret_gn_g": g("ret_gn_g"), "subln_g": g("diff_subln_g"), "lam4": np.ascontiguousarray(lam4),
        "w_ret_out": g("w_ret_out")[0], "w_diff_out": g("w_diff_out")[0], "w_o": g("w_o")[0],
        "biasT": biasT, "bfar": bfar, "w_router": g("w_router")[0],
        "w_gate": g("w_exp_gate")[0], "w_up": g("w_exp_up")[0], "w_down": g("w_exp_down")[0],
        "cosT": tabs["cosT"], "sinT": tabs["sinT"], "maskT": tabs["maskT"], "kdec": tabs["kdec"],
        "qdec": tabs["qdec"], "ident": tabs["ident"], "iota": tabs["iota"], "pt": tabs["pt"],
    }
    in_maps = []
    for b in range(B):
        m = dict(shared)
        m["x"] = np.ascontiguousarray(x[b])
        m["cT"] = np.ascontiguousarray(c[b].reshape(8, 128).T)
        in_maps.append(m)
    return in_maps


def run(inputs, stop_after=None, dbg=None, trace=False, cores=8):
    global _TABS
    if _TABS is None:
        _TABS = _host_tables()
    nc, dbg_out = build_program(_TABS, stop_after=stop_after, dbg=dbg)
    in_maps = _prep_inputs(inputs, _TABS)[:cores]
    used = set(nc_input_names(nc))
    in_maps = [{k: v for k, v in m.items() if k in used} for m in in_maps]
    res = run_bass_kernel_spmd(nc, in_maps, core_ids=list(range(len(in_maps))), trace=trace)
    return res


def nc_input_names(nc):
    return _INPUT_NAMES


_INPUT_NAMES = ["x", "cT", "w_ada", "b_ada", "g1T", "g2T", "final_g", "w_in", "ret_gn_g", "subln_g", "lam4",
                "w_ret_out", "w_diff_out", "w_o", "biasT", "bfar", "w_router", "w_gate", "w_up", "w_down",
                "cosT", "sinT", "maskT", "kdec", "qdec", "ident", "iota", "pt"]


def kernel(**inputs):
    res = run(inputs)
    out = np.stack([np.asarray(r["out"], dtype=np.float32) for r in res.results], axis=0)
    return out
```
